# Optimizing a Trainium2 kernel written in Bass

```python
import math
import jax
import jax.numpy as jnp
from jax import lax
import numpy as np

D_MODEL = 2048
BATCH = 4
SEQ = 4096
DEPTH = 4

MIX_WIDTH = D_MODEL // 2
POOL_WINDOWS = (2, 4, 8, 16)
POOL_GROUPS = len(POOL_WINDOWS)
POOL_GROUP_DIM = MIX_WIDTH // POOL_GROUPS
DIFF_HEADS = 8
DIFF_QK_DIM = MIX_WIDTH // (2 * DIFF_HEADS)
DIFF_V_DIM = MIX_WIDTH // DIFF_HEADS
Q_BLOCK = 128
RET_HEADS = 8
RET_K_DIM = MIX_WIDTH // RET_HEADS
RET_V_DIM = MIX_WIDTH // RET_HEADS
RET_CHUNK = 128
ROPE_BASE = 10000.0
N_BRANCHES = 3
D_FF = 4 * D_MODEL
REL_BUCKETS = 32
REL_MAX_DIST = 128
NORM_EPS = 1e-6
NEG_INF = -1e30
IN_SPLITS = (MIX_WIDTH,) * 8 + (N_BRANCHES * D_MODEL,)
IN_COLS = sum(IN_SPLITS)

kernel_name = 'hybrid_gated_pool_diffattn_retention_block'


def rmsnorm(x, gain=None):
    xf = x.astype(jnp.float32)
    y = xf * lax.rsqrt(jnp.mean(xf * xf, axis=-1, keepdims=True) + NORM_EPS)
    if gain is not None:
        y = y * gain.astype(jnp.float32)
    return y.astype(x.dtype)


def t5_causal_bucket(q_pos, k_pos):
    n = jnp.maximum(q_pos[:, None] - k_pos[None, :], 0)
    exact = REL_BUCKETS // 2
    nf = jnp.maximum(n, 1).astype(jnp.float32)
    large = exact + (jnp.log(nf / exact) / math.log(REL_MAX_DIST / exact)
                     * (REL_BUCKETS - exact)).astype(jnp.int32)
    large = jnp.minimum(large, REL_BUCKETS - 1)
    return jnp.where(n < exact, n, large)


def rotary(t):
    S, half = t.shape[1], t.shape[-1] // 2
    inv_freq = 1.0 / (ROPE_BASE ** jnp.linspace(0.0, 1.0, half, dtype=jnp.float32))
    ang = jnp.arange(S, dtype=jnp.float32)[:, None] * inv_freq[None, :]
    cos = jnp.cos(ang)[None, :, None, :]
    sin = jnp.sin(ang)[None, :, None, :]
    t1, t2 = t[..., :half], t[..., half:]
    return jnp.concatenate([t1 * cos - t2 * sin, t2 * cos + t1 * sin], axis=-1)


def pool_mixer(u, pool_w, pool_scale):
    B, S, _ = u.shape
    uf = u.astype(jnp.float32).reshape(B, S, POOL_GROUPS, POOL_GROUP_DIM)
    cs = jnp.pad(jnp.cumsum(uf, axis=1), ((0, 0), (1, 0), (0, 0), (0, 0)))
    t = jnp.arange(S)
    win = jnp.array(POOL_WINDOWS, jnp.int32)
    lo = jnp.maximum(t[:, None] + 1 - win[None, :], 0)
    g_idx = jnp.arange(POOL_GROUPS)[None, :]
    lower = cs[:, lo, g_idx, :]
    count = (t[:, None] + 1 - lo).astype(jnp.float32)
    delta = (cs[:, 1:] - lower) / count[None, :, :, None] - uf
    y = jnp.einsum('bsgc,gcd->bsgd', delta.astype(u.dtype), pool_w)
    return y.reshape(B, S, MIX_WIDTH) * pool_scale


def diff_attention(q, k, v, lam_params, head_gain, rel_bias, lambda_init):
    B, S, _ = q.shape
    nb = S // Q_BLOCK
    q = q.reshape(B, S, DIFF_HEADS, 2, DIFF_QK_DIM).transpose(0, 2, 3, 1, 4)
    k = k.reshape(B, S, DIFF_HEADS, 2, DIFF_QK_DIM).transpose(0, 2, 3, 1, 4)
    v = v.reshape(B, S, DIFF_HEADS, DIFF_V_DIM).transpose(0, 2, 1, 3)
    lp = lam_params.astype(jnp.float32)
    lam = jnp.exp(jnp.sum(lp[0] * lp[1])) - jnp.exp(jnp.sum(lp[2] * lp[3])) + lambda_init
    scale = DIFF_QK_DIM ** -0.5
    k_pos = jnp.arange(S)
    bias_table = rel_bias.astype(jnp.float32)
    q_blocks = q.reshape(B, DIFF_HEADS, 2, nb, Q_BLOCK, DIFF_QK_DIM).transpose(3, 0, 1, 2, 4, 5)

    def block(args):
        qb, blk = args
        q_pos = blk * Q_BLOCK + jnp.arange(Q_BLOCK)
        bias = bias_table[t5_causal_bucket(q_pos, k_pos)].transpose(2, 0, 1)
        logits = jnp.einsum('bhmqd,bhmkd->bhmqk', qb, k).astype(jnp.float32) * scale
        logits = logits + bias[None, :, None]
        causal = k_pos[None, :] <= q_pos[:, None]
        p = jax.nn.softmax(jnp.where(causal, logits, NEG_INF), axis=-1)
        attn = p[:, :, 0] - lam * p[:, :, 1]
        return jnp.einsum('bhqk,bhkd->bhqd', attn.astype(v.dtype), v)

    out = lax.map(block, (q_blocks, jnp.arange(nb)))
    out = out.transpose(1, 2, 0, 3, 4).reshape(B, DIFF_HEADS, S, DIFF_V_DIM)
    out = rmsnorm(out, head_gain) * (1.0 - lambda_init)
    return out.transpose(0, 2, 1, 3).reshape(B, S, MIX_WIDTH)


def retention(q, k, v, g):
    B, S, _ = q.shape
    nc = S // RET_CHUNK
    f32 = jnp.float32
    qh = rotary(q.astype(f32).reshape(B, S, RET_HEADS, RET_K_DIM))
    kh = rotary(k.astype(f32).reshape(B, S, RET_HEADS, RET_K_DIM)) * RET_K_DIM ** -0.5
    vh = v.astype(f32).reshape(B, S, RET_HEADS, RET_V_DIM)

    def chunks(t):
        return t.reshape(B, nc, RET_CHUNK, RET_HEADS, t.shape[-1]).transpose(1, 0, 3, 2, 4)

    log_gamma = jnp.log(1.0 - 2.0 ** (-5.0 - jnp.arange(RET_HEADS, dtype=f32)))
    pos = jnp.arange(RET_CHUNK, dtype=f32)
    rel = pos[:, None] - pos[None, :]
    intra = jnp.where(rel >= 0, jnp.exp(jnp.maximum(rel, 0.0) * log_gamma[:, None, None]), 0.0)
    key_decay = jnp.exp((RET_CHUNK - 1 - pos)[None, :] * log_gamma[:, None])
    query_decay = jnp.exp((pos + 1)[None, :] * log_gamma[:, None])
    chunk_decay = jnp.exp(RET_CHUNK * log_gamma)

    def step(state, qkv):
        qc, kc, vc = qkv
        scores = jnp.einsum('bhid,bhjd->bhij', qc, kc) * intra
        inner = jnp.einsum('bhij,bhje->bhie', scores, vc)
        cross = jnp.einsum('bhid,bhde->bhie', qc, state) * query_decay[..., None]
        state = chunk_decay[:, None, None] * state + jnp.einsum(
            'bhjd,bhje->bhde', kc * key_decay[..., None], vc)
        return state, inner + cross

    state0 = jnp.zeros((B, RET_HEADS, RET_K_DIM, RET_V_DIM), f32)
    _, out = lax.scan(step, state0, (chunks(qh), chunks(kh), chunks(vh)))
    out = out.transpose(1, 0, 3, 2, 4).reshape(B, S, RET_HEADS, RET_V_DIM)
    out = rmsnorm(out).reshape(B, S, MIX_WIDTH)
    return (jax.nn.silu(g.astype(f32)) * out).astype(g.dtype)


def setup_inputs(seed: int = 0) -> dict:
    key = jax.random.key(seed)
    ks = jax.random.split(key, 15)
    f32 = jnp.float32

    def normal(k, shape, scale):
        return jax.random.normal(k, shape, f32) * scale

    def gain(k, shape):
        return 1.0 + 0.02 * jax.random.normal(k, shape, f32)

    return {
        'x': normal(ks[0], (BATCH, SEQ, D_MODEL), 1.0),
        'rel_bias': normal(ks[1], (REL_BUCKETS, DIFF_HEADS), 0.5),
        'norm_pre_mix': gain(ks[2], (DEPTH, D_MODEL)),
        'norm_post_mix': gain(ks[3], (DEPTH, D_MODEL)),
        'norm_pre_mlp': gain(ks[4], (DEPTH, D_MODEL)),
        'norm_post_mlp': gain(ks[5], (DEPTH, D_MODEL)),
        'w_in': normal(ks[6], (DEPTH, D_MODEL, IN_COLS), D_MODEL ** -0.5),
        'pool_w': normal(ks[7], (DEPTH, POOL_GROUPS, POOL_GROUP_DIM, POOL_GROUP_DIM), POOL_GROUP_DIM ** -0.5),
        'pool_scale': gain(ks[8], (DEPTH, MIX_WIDTH)),
        'diff_lambda': normal(ks[9], (DEPTH, 4, DIFF_QK_DIM), 0.1),
        'diff_head_norm': gain(ks[10], (DEPTH, DIFF_V_DIM)),
        'w_branch': normal(ks[11], (DEPTH, N_BRANCHES, MIX_WIDTH, D_MODEL), MIX_WIDTH ** -0.5),
        'w_out': normal(ks[12], (DEPTH, D_MODEL, D_MODEL), D_MODEL ** -0.5),
        'w_up': normal(ks[13], (DEPTH, D_MODEL, D_FF), D_MODEL ** -0.5),
        'w_down': normal(ks[14], (DEPTH, D_FF, D_MODEL), D_FF ** -0.5),
    }


def reference(x, rel_bias, norm_pre_mix, norm_post_mix, norm_pre_mlp, norm_post_mlp,
              w_in, pool_w, pool_scale, diff_lambda, diff_head_norm, w_branch, w_out,
              w_up, w_down):
    B, S, _ = x.shape
    split_at = [int(i) for i in np.cumsum(IN_SPLITS)[:-1]]
    for l in range(DEPTH):
        lambda_init = 0.8 - 0.6 * math.exp(-0.3 * l)
        a = rmsnorm(x, norm_pre_mix[l])
        proj = jnp.einsum('bsd,dc->bsc', a, w_in[l])
        u_pool, q_d, k_d, v_d, q_r, k_r, v_r, g_r, gate_logits = jnp.split(proj, split_at, axis=-1)
        y_pool = pool_mixer(u_pool, pool_w[l], pool_scale[l])
        y_diff = diff_attention(q_d, k_d, v_d, diff_lambda[l], diff_head_norm[l], rel_bias, lambda_init)
        y_ret = retention(q_r, k_r, v_r, g_r)
        branches = jnp.stack([y_pool, y_diff, y_ret], axis=2)
        widened = jnp.einsum('bsnc,ncd->bsnd', branches, w_branch[l])
        gates = jax.nn.sigmoid(gate_logits.reshape(B, S, N_BRANCHES, D_MODEL))
        merged = jnp.sum(gates * widened, axis=2)
        mix = jnp.einsum('bsd,de->bse', merged, w_out[l])
        x = x + rmsnorm(mix, norm_post_mix[l])
        h = rmsnorm(x, norm_pre_mlp[l])
        ff = jnp.square(jax.nn.relu(jnp.einsum('bsd,df->bsf', h, w_up[l])))
        ff = jnp.einsum('bsf,fd->bsd', ff, w_down[l])
        x = x + rmsnorm(ff, norm_post_mlp[l])
    return x
```

```python
import math
import numpy as np
from contextlib import ExitStack
import concourse.bass as bass
import concourse.mybir as mybir
from concourse.bass_utils import run_bass_kernel_spmd

F32 = mybir.dt.float32
BF16 = mybir.dt.bfloat16
AF = mybir.ActivationFunctionType
ALU = mybir.AluOpType

D = 2048
S = 4096
TH = 2048
NHALF = 2
DEPTH = 4
MIX = 1024
DFF = 8192
INC = 14336
EPS = 1e-6
ENGS = ("pe", "act", "dve", "pool", "sp")


class Op:
    __slots__ = ("eng", "fn", "deps", "signal", "chan", "cidx", "sig", "waits", "know", "gid", "inc")


class Prog:
    def __init__(self):
        self.ops = {e: [] for e in ENGS}
        self.all = []
        self.last_w = {}
        self.readers = {}
        self.chan_last = {}
        self.chan_cnt = {}
        self.bar = {e: [] for e in ENGS}

    def add(self, eng, fn, r=(), w=(), chan=None, inc=16):
        op = Op()
        op.eng = eng
        op.fn = fn
        op.chan = chan
        op.signal = False
        op.cidx = 0
        deps = set()
        for k in r:
            lw = self.last_w.get(k)
            if lw is not None:
                deps.add(lw)
        for k in w:
            lw = self.last_w.get(k)
            if lw is not None:
                deps.add(lw)
            for rd in self.readers.get(k, ()):
                deps.add(rd)
        if chan is not None:
            cl = self.chan_last.get(chan)
            if cl is not None:
                deps.add(cl)
            self.chan_last[chan] = op
            op.cidx = self.chan_cnt.get(chan, 0) + inc
            self.chan_cnt[chan] = op.cidx
            op.inc = inc
        for d in self.bar[eng]:
            deps.add(d)
        self.bar[eng] = []
        deps.discard(op)
        if eng == "pe":
            deps = {d for d in deps if not (d.eng == "pe" and d.chan is None)}
        op.deps = deps
        for k in w:
            self.last_w[k] = op
            self.readers[k] = []
        for k in r:
            self.readers.setdefault(k, []).append(op)
        op.gid = len(self.all)
        self.all.append(op)
        self.ops[eng].append(op)
        return op

    def barrier(self):
        lst = []
        for e in ENGS:
            for op in reversed(self.ops[e]):
                if op.chan is None and op.fn is not None:
                    lst.append(op)
                    break
        for c, op in self.chan_last.items():
            lst.append(op)
        for e in ENGS:
            self.bar[e] = list(lst)

    def lower(self):
        for op in self.all:
            for d in op.deps:
                d.signal = True
        cnt = {e: 0 for e in ENGS}
        know = {e: {} for e in ENGS}
        for op in self.all:
            e = op.eng
            waits = {}
            kn = know[e]
            for d in op.deps:
                if d.chan is not None:
                    key, val = ("c", d.chan), d.cidx
                else:
                    key, val = ("e", d.eng), d.sig
                if kn.get(key, 0) >= val:
                    continue
                if waits.get(key, 0) < val:
                    waits[key] = val
                new = dict(kn)
                for k2, v2 in d.know.items():
                    if new.get(k2, 0) < v2:
                        new[k2] = v2
                kn = new
            know[e] = kn
            op.waits = list(waits.items())
            if op.chan is not None:
                kk = dict(kn)
                kk[("c", op.chan)] = op.cidx
                op.know = kk
                op.sig = 0
            elif op.signal:
                cnt[e] += 1
                op.sig = cnt[e]
                kk = dict(kn)
                kk[("e", e)] = op.sig
                op.know = kk
            else:
                op.sig = 0
                op.know = kn

    def emit(self, nc, block, esem, csem):
        def run(ename):
            def body(eng):
                for op in self.ops[ename]:
                    for key, val in op.waits:
                        sem = csem[key[1]] if key[0] == "c" else esem[key[1]]
                        eng.wait_ge(sem, val)
                    if op.fn is None:
                        continue
                    ins = op.fn(eng)
                    if op.chan is not None:
                        ins.then_inc(csem[op.chan], op.inc) if op.inc != 1 else ins.then_inc(csem[op.chan])
                    elif op.signal:
                        ins.then_inc(esem[ename], 1)
            return body
        block.tensor(run("pe"))
        block.scalar(run("act"))
        block.vector(run("dve"))
        block.gpsimd(run("pool"))
        block.sync(run("sp"))


class SBAlloc:
    def __init__(self, nc, base=20480, limit=204800):
        self.nc = nc
        self.off = base
        self.limit = limit
        self.n = 0

    def alloc(self, shape, dtype):
        nb = 4 if dtype == F32 else 2
        sz = nb
        for s_ in shape[1:]:
            sz *= s_
        sz = (sz + 63) // 64 * 64
        t = self.nc.alloc_sbuf_tensor_at("sb%d" % self.n, list(shape), dtype, offset=self.off)
        self.n += 1
        self.off += sz
        assert self.off <= self.limit, ("SBUF overflow", self.off)
        return t


def lambda_init(l):
    return 0.8 - 0.6 * math.exp(-0.3 * l)


def build_program(n_layers=DEPTH, debug=False):
    nc = bass.Bass("TRN2", target_bir_lowering=False)
    P = Prog()

    def din(name, shape, dt=F32):
        return nc.dram_tensor(name, list(shape), dt, kind="ExternalInput").ap()

    x_in = din("x", [TH, D])
    w_in = din("w_in", [DEPTH, D, INC])
    pool_w = din("pool_w", [DEPTH, 4, 256, 256])
    w_branch = din("w_branch", [DEPTH, 3, MIX, D])
    w_out = din("w_out", [DEPTH, D, D])
    w_up = din("w_up", [DEPTH, D, DFF])
    w_down = din("w_down", [DEPTH, DFF, D])
    c_gains = din("c_gains", [128, 4 * DEPTH * 16])
    c_pscale = din("c_pscale", [128, DEPTH * 8])
    c_hgain = din("c_hgain", [128, DEPTH])
    c_lam = din("c_lam", [128, DEPTH * 256])
    c_bfar = din("c_bfar", [128, 8])
    c_wbias = din("c_wbias", [128, 8 * 1024])
    c_cos = din("c_cos", [128, TH])
    c_sin = din("c_sin", [128, TH])
    c_dec = din("c_dec", [128, 8 * 2 * 128])
    c_corr = din("c_corr", [128, 4 * 16])
    c_mask = din("c_mask", [128, 512])
    c_ident = din("c_ident", [128, 128])
    c_swap = din("c_swap", [128, 128])
    c_hflag = din("c_hflag", [128, 1])
    c_bfarh = din("c_bfarh", [128, 8])
    c_wbiash = din("c_wbiash", [128, 8 * 512])
    out = nc.dram_tensor("out", [TH, D], F32, kind="ExternalOutput").ap()

    def dscr(name, shape, dt):
        kind = "ExternalOutput" if (debug and name in ("s_yT", "s_xT")) else "Internal"
        return nc.dram_tensor(name, list(shape), dt, kind=kind).ap()

    xT = dscr("s_xT", [16, 128, TH], F32)
    mixT = dscr("s_mixT", [16, 128, TH], F32)
    uT = dscr("s_uT", [8, 128, TH], F32)
    qdT = dscr("s_qdT", [8, 128, TH], BF16)
    kdT = dscr("s_kdT", [8, 128, TH], BF16)
    vd = dscr("s_vd", [TH, MIX], BF16)
    qrT = dscr("s_qrT", [8, 128, TH], BF16)
    krT = dscr("s_krT", [8, 128, TH], BF16)
    vr = dscr("s_vr", [TH, MIX], BF16)
    grT = dscr("s_grT", [8, 128, TH], BF16)
    gaT = dscr("s_gaT", [48, 128, TH], BF16)
    yT = dscr("s_yT", [24, 128, TH], BF16)
    mgT = dscr("s_mgT", [16, 128, TH], BF16)
    ffT = dscr("s_ffT", [64, 128, TH], BF16)
    rst = dscr("s_rst", [8, 128, 128], F32)
    utl = dscr("s_utl", [128, 128], F32)
    gk_all = [[dscr("g_k%d_%d" % (j, par), [2048, 1024], BF16) for j in range(2)] for par in range(2)]
    gv_all = [[dscr("g_v%d_%d" % (j, par), [2048, 1024], BF16) for j in range(2)] for par in range(2)]
    gst_all = [dscr("g_st_%d" % par, [2048, 128], F32) for par in range(2)]
    gtl_all = [dscr("g_tl_%d" % par, [256, 128], F32) for par in range(2)]

    sb = SBAlloc(nc)
    ident = sb.alloc([128, 128], F32)
    identb = sb.alloc([128, 128], BF16)
    swapb = sb.alloc([128, 128], BF16)
    onesb = sb.alloc([128, 128], BF16)
    gains = sb.alloc([128, 4, DEPTH, 16], F32)
    pscale = sb.alloc([128, DEPTH, 8], F32)
    hgain = sb.alloc([128, DEPTH], F32)
    lamt = sb.alloc([128, DEPTH, 4, 64], F32)
    bfar = sb.alloc([128, 8], F32)
    neglam = sb.alloc([128, DEPTH], F32)
    lamtmp = sb.alloc([128, 2, 64], F32)
    lamred = sb.alloc([128, 2], F32)
    epsc = sb.alloc([128, 1], F32)
    hflag = sb.alloc([128, 1], F32)
    bfarh = sb.alloc([128, 8], F32)
    PH0 = sb.off

    stack = ExitStack()
    ps = stack.enter_context(nc.psum_tensor("ps", [128, 8, 512], F32))
    esem = {e: stack.enter_context(nc.semaphore("se_" + e)) for e in ENGS}

    psn = [0]

    def bank():
        b = psn[0] % 8
        psn[0] += 1
        return b

    def PS(b):
        return ("ps", b)

    def dma(eng, out_ap, in_ap, chan, r=(), w=()):
        return P.add(eng, lambda e: e.dma_start(out=out_ap, in_=in_ap), r=r, w=w, chan=chan)

    ev_rr = [0]

    def evac_copy(out_ap, in_ap, r, w, eng=None):
        if eng is None:
            eng = ("act", "dve")[ev_rr[0] % 2]
            ev_rr[0] += 1
        if eng == "act":
            return P.add("act", lambda e: e.activation(out=out_ap, in_=in_ap, func=AF.Copy), r=r, w=w)
        return P.add(eng, lambda e: e.tensor_copy(out=out_ap, in_=in_ap), r=r, w=w)

    P.add("dve", lambda e: e.memset(onesb[:], 1.0), w=["onesb"])
    P.add("dve", lambda e: e.memset(epsc[:], EPS), w=["epsc"])
    dma("sp", ident[:], c_ident[:, :], "c0", w=["ident"])
    dma("pool", identb[:], c_ident[:, :], "c1", w=["identb"])
    dma("pool", swapb[:], c_swap[:, :], "c1", w=["swapb"])
    dma("sp", gains[:].rearrange("p a b c -> p (a b c)"), c_gains[:, :], "c0", w=["gains"])
    dma("sp", pscale[:].rearrange("p a b -> p (a b)"), c_pscale[:, :], "c0", w=["pscale"])
    dma("sp", hgain[:], c_hgain[:, :], "c0", w=["hgain"])
    dma("sp", lamt[:].rearrange("p a b c -> p (a b c)"), c_lam[:, :], "c0", w=["lamt"])
    dma("sp", bfar[:], c_bfar[:, :], "c0", w=["bfar"])
    dma("sp", bfarh[:], c_bfarh[:, :], "c0", w=["bfarh"])
    dma("sp", hflag[:], c_hflag[:, :], "c0", w=["hflag"])
    for l in range(n_layers):
        P.add("dve", lambda e, l=l: e.tensor_tensor(out=lamtmp[:, 0, :], in0=lamt[:, l, 0, :], in1=lamt[:, l, 1, :], op=ALU.mult), r=["lamt"], w=["lamtmp0"])
        P.add("dve", lambda e, l=l: e.tensor_tensor(out=lamtmp[:, 1, :], in0=lamt[:, l, 2, :], in1=lamt[:, l, 3, :], op=ALU.mult), r=["lamt"], w=["lamtmp1"])
        P.add("dve", lambda e: e.reduce_sum(out=lamred[:], in_=lamtmp[:], axis=mybir.AxisListType.X), r=["lamtmp0", "lamtmp1"], w=["lamred"])
        P.add("act", lambda e: e.activation(out=lamred[:], in_=lamred[:], func=AF.Exp), r=["lamred"], w=["lamred"])
        P.add("dve", lambda e, l=l: e.tensor_tensor(out=neglam[:, l:l + 1], in0=lamred[:, 1:2], in1=lamred[:, 0:1], op=ALU.subtract), r=["lamred"], w=["neglam"])
        P.add("dve", lambda e, l=l: e.tensor_scalar_add(out=neglam[:, l:l + 1], in0=neglam[:, l:l + 1], scalar1=-lambda_init(l)), r=["neglam"], w=["neglam"])
    P.barrier()

    def phase_transpose_in():
        sb.off = PH0
        xin = [sb.alloc([128, 4, D], F32) for _ in range(2)]
        stg = [sb.alloc([128, 16, 512], F32) for _ in range(2)]
        for tg in range(TH // 512):
            s_ = tg % 2
            dma("sp", xin[s_][:], x_in[tg * 512:(tg + 1) * 512, :].rearrange("(j p) d -> p j d", p=128), "ti_in%d" % s_, w=[("xin", s_)])
            for k in range(16):
                b = bank()

                def tr(e, k=k, b=b, s_=s_):
                    ins = None
                    for j in range(4):
                        ins = e.transpose(out=ps[:, b, j * 128:(j + 1) * 128], in_=xin[s_][:, j, k * 128:(k + 1) * 128], identity=ident[:])
                    return ins
                P.add("pe", tr, r=[("xin", s_)], w=[PS(b)])
                evac_copy(stg[s_][:, k, :], ps[:, b, :], r=[PS(b)], w=[("tstg", s_, k)])
            dma("sp", xT[:, :, tg * 512:(tg + 1) * 512].rearrange("k p t -> p k t"), stg[s_][:], "ti_out%d" % s_, r=[("tstg", s_, k) for k in range(16)])
        P.barrier()

    def phase_transpose_out():
        sb.off = PH0
        xin = [sb.alloc([128, 16, 512], F32) for _ in range(2)]
        stg = [sb.alloc([128, 4, D], F32) for _ in range(2)]
        for tg in range(TH // 512):
            s_ = tg % 2
            dma("sp", xin[s_][:], xT[:, :, tg * 512:(tg + 1) * 512].rearrange("k p t -> p k t"), "to_in%d" % s_, w=[("xin", s_)])
            for j in range(4):
                for kg in range(4):
                    b = bank()

                    def tr(e, j=j, kg=kg, b=b, s_=s_):
                        ins = None
                        for kk in range(4):
                            k = kg * 4 + kk
                            ins = e.transpose(out=ps[:, b, kk * 128:(kk + 1) * 128], in_=xin[s_][:, k, j * 128:(j + 1) * 128], identity=ident[:])
                        return ins
                    P.add("pe", tr, r=[("xin", s_)], w=[PS(b)])
                    evac_copy(stg[s_][:, j, kg * 512:(kg + 1) * 512], ps[:, b, :], r=[PS(b)], w=[("tstg", s_, j, kg)])
            dma("sp", out[tg * 512:(tg + 1) * 512, :].rearrange("(j p) d -> p j d", p=128), stg[s_][:], "to_out%d" % s_,
                r=[("tstg", s_, j, kg) for j in range(4) for kg in range(4)], w=["OUT"])
        P.barrier()

    def stats_block(blk, blk_key, sqb, rstd_ap, rstd_key, nd):
        b = bank()
        sqa, sqk = sqb
        P.add("act", lambda e: e.activation(out=sqa[:, 0:10, :], in_=blk[:, 0:10, :], func=AF.Square), r=[blk_key], w=[(sqk, 0)])
        P.add("pool", lambda e: e.tensor_tensor(out=sqa[:, 10:16, :], in0=blk[:, 10:16, :], in1=blk[:, 10:16, :], op=ALU.mult), r=[blk_key], w=[(sqk, 1)])

        def mmq(e, b=b):
            ins = None
            for k in range(16):
                ins = e.matmul(ps[:, b, :], lhsT=onesb[:], rhs=sqa[:, k, :], start=(k == 0), stop=(k == 15))
            return ins
        P.add("pe", mmq, r=[(sqk, 0), (sqk, 1)], w=[PS(b)])
        P.add("act", lambda e, b=b: e.activation(out=rstd_ap, in_=ps[:, b, :], func=AF.Sqrt, scale=1.0 / nd, bias=epsc[:]), r=[PS(b)], w=[rstd_key])
        P.add("dve", lambda e: e.reciprocal(out=rstd_ap, in_=rstd_ap), r=[rstd_key], w=[rstd_key])

    def phase_prenorm(hf, l, norm_idx, dst, dst_key):
        mark = sb.off
        xb = [sb.alloc([128, 16, 512], F32) for _ in range(2)]
        sqb = [sb.alloc([128, 16, 512], BF16) for _ in range(2)]
        rstd = [sb.alloc([128, 512], F32) for _ in range(2)]
        for tb in range(4):
            s_ = tb % 2
            t0 = hf * TH + tb * 512
            dma("sp", xb[s_][:], xT[:, :, t0:t0 + 512].rearrange("k p t -> p k t"), "pn_in%d" % s_, w=[("xb", s_)])
            stats_block(xb[s_], ("xb", s_), (sqb[s_], ("sqa", s_)), rstd[s_][:], ("rstd", s_), D)
            for k in range(16):
                eng = "dve"
                P.add(eng, lambda e, k=k, s_=s_, tb=tb: e.scalar_tensor_tensor(
                    out=dst[:, k, tb * 512:(tb + 1) * 512], in0=xb[s_][:, k, :], scalar=gains[:, norm_idx, l, k:k + 1],
                    in1=rstd[s_][:], op0=ALU.mult, op1=ALU.mult), r=[("xb", s_), ("rstd", s_)], w=[(dst_key, k, tb)])
        sb.off = mark
        P.barrier()

    def phase_postnorm(hf, l, norm_idx):
        sb.off = PH0
        xb = [sb.alloc([128, 16, 512], F32) for _ in range(2)]
        mb = [sb.alloc([128, 16, 512], F32) for _ in range(2)]
        sqb = [sb.alloc([128, 16, 512], BF16) for _ in range(2)]
        rstd = [sb.alloc([128, 512], F32) for _ in range(2)]
        for tb in range(4):
            s_ = tb % 2
            t0 = hf * TH + tb * 512
            dma("sp", mb[s_][:], mixT[:, :, t0:t0 + 512].rearrange("k p t -> p k t"), "po_m%d" % s_, w=[("mb", s_)])
            dma("sp", xb[s_][:], xT[:, :, t0:t0 + 512].rearrange("k p t -> p k t"), "po_x%d" % s_, w=[("xb", s_)])
            stats_block(mb[s_], ("mb", s_), (sqb[s_], ("sqa", s_)), rstd[s_][:], ("rstd", s_), D)
            for k in range(16):
                eng = "pool"
                P.add("dve", lambda e, k=k, s_=s_: e.scalar_tensor_tensor(
                    out=mb[s_][:, k, :], in0=mb[s_][:, k, :], scalar=gains[:, norm_idx, l, k:k + 1],
                    in1=rstd[s_][:], op0=ALU.mult, op1=ALU.mult), r=[("mb", s_), ("rstd", s_)], w=[("mb", s_)])
                P.add(eng, lambda e, k=k, s_=s_: e.tensor_tensor(
                    out=xb[s_][:, k, :], in0=xb[s_][:, k, :], in1=mb[s_][:, k, :], op=ALU.add), r=[("mb", s_), ("xb", s_)], w=[("xb", s_)])
            dma("sp", xT[:, :, t0:t0 + 512].rearrange("k p t -> p k t"), xb[s_][:], "po_o%d" % s_, r=[("xb", s_)])
        P.barrier()

    wld = [0]

    def load_w(wbuf, slot, src_ap):
        wld[0] += 1
        return dma("pool", wbuf[slot][:], src_ap.rearrange("(k p) c -> p k c", p=128), "w%d" % slot, w=[("wbuf", slot)])

    def phase_inproj(hf, l):
        sb.off = PH0
        aT = sb.alloc([128, 16, TH], BF16)
        phase_prenorm(hf, l, 0, aT, "aT")
        wbuf = [sb.alloc([128, 16, 512], BF16) for _ in range(2)]
        stf = [sb.alloc([128, TH], F32) for _ in range(2)]
        stb = [sb.alloc([128, TH], BF16) for _ in range(2)]
        aT_keys = [("aT", k, tb) for k in range(16) for tb in range(4)]
        nst = [0]
        h0 = hf * TH
        for cb in range(28):
            slot = cb % 2
            load_w(wbuf, slot, w_in[l, :, cb * 512:(cb + 1) * 512])
            seg = cb // 2 if cb < 16 else 8
            if seg in (3, 6):
                dst = vd if seg == 3 else vr
                c0 = (cb % 2) * 512
                for tg in range(4):
                    si = nst[0] % 2
                    nst[0] += 1
                    for j in range(4):
                        tt = tg * 4 + j
                        b = bank()

                        def mm(e, tt=tt, b=b, slot=slot):
                            ins = None
                            for k in range(16):
                                ins = e.matmul(ps[:, b, :], lhsT=aT[:, k, tt * 128:(tt + 1) * 128], rhs=wbuf[slot][:, k, :], start=(k == 0), stop=(k == 15))
                            return ins
                        P.add("pe", mm, r=aT_keys[tt // 4::4] + [("wbuf", slot)], w=[PS(b)])
                        evac_copy(stb[si][:, j * 512:(j + 1) * 512], ps[:, b, :], r=[PS(b)], w=[("stb", si, j)])
                    r0 = h0 + tg * 512
                    dma("sp", dst[r0:r0 + 512, c0:c0 + 512].rearrange("(j p) c -> p j c", p=128),
                        stb[si][:].rearrange("p (j c) -> p j c", j=4), "ip_sb%d" % si, r=[("stb", si, j) for j in range(4)])
                continue
            for sub in range(4):
                col = cb * 512 + sub * 128
                si = nst[0] % 2
                nst[0] += 1
                for tb in range(4):
                    b = bank()

                    def mm(e, tb=tb, b=b, slot=slot, sub=sub):
                        ins = None
                        for k in range(16):
                            ins = e.matmul(ps[:, b, :], lhsT=wbuf[slot][:, k, sub * 128:(sub + 1) * 128], rhs=aT[:, k, tb * 512:(tb + 1) * 512], start=(k == 0), stop=(k == 15))
                        return ins
                    P.add("pe", mm, r=aT_keys[tb::4] + [("wbuf", slot)], w=[PS(b)])
                    if seg == 0:
                        evac_copy(stf[si][:, tb * 512:(tb + 1) * 512], ps[:, b, :], r=[PS(b)], w=[("stf", si, tb)])
                    elif seg == 7:
                        P.add("act", lambda e, si=si, tb=tb, b=b: e.activation(out=stb[si][:, tb * 512:(tb + 1) * 512], in_=ps[:, b, :], func=AF.Silu), r=[PS(b)], w=[("stb", si, tb)])
                    elif seg == 8:
                        P.add("act", lambda e, si=si, tb=tb, b=b: e.activation(out=stb[si][:, tb * 512:(tb + 1) * 512], in_=ps[:, b, :], func=AF.Sigmoid), r=[PS(b)], w=[("stb", si, tb)])
                    else:
                        evac_copy(stb[si][:, tb * 512:(tb + 1) * 512], ps[:, b, :], r=[PS(b)], w=[("stb", si, tb)])
                if seg == 0:
                    dma("sp", uT[col // 128, :, h0:h0 + TH], stf[si][:], "ip_sf%d" % si, r=[("stf", si, tb) for tb in range(4)])
                else:
                    if seg == 8:
                        dstap = gaT[(col - 8192) // 128, :, h0:h0 + TH]
                    else:
                        dt_ = {1: qdT, 2: kdT, 4: qrT, 5: krT, 7: grT}[seg]
                        dstap = dt_[(col - seg * 1024) // 128, :, h0:h0 + TH]
                    dma("sp", dstap, stb[si][:], "ip_sb%d" % si, r=[("stb", si, tb) for tb in range(4)])
        P.barrier()

    def phase_pool(hf, l):
        sb.off = PH0
        gtl = gtl_all[l % 2]
        pw = sb.alloc([128, 4, 2, 256], BF16)
        corr = sb.alloc([128, 4, 16], F32)
        uas = [sb.alloc([128, 2, 16 + TH], F32) for _ in range(2)]
        ubs = [sb.alloc([128, 2, 16 + TH], F32) for _ in range(2)]
        ucs = [sb.alloc([128, 2, 16 + TH], F32) for _ in range(2)]
        dls = [sb.alloc([128, 2, TH], BF16) for _ in range(2)]
        stb = [sb.alloc([128, TH], BF16) for _ in range(2)]
        h0 = hf * TH
        for g in range(4):
            dma("pool", pw[:, g], pool_w[l, g].rearrange("(k p) c -> p k c", p=128), "pl_w", w=[("pw", g)])
        dma("sp", corr[:].rearrange("p a b -> p (a b)"), c_corr[:, :], "pl_c", w=["corr"])
        nst = 0

        def load_group(g):
            par = g % 2
            ua = uas[par]
            dma("sp", ua[:, :, 0:16], gtl[0:128, g * 32:(g + 1) * 32].rearrange("p (k t) -> p k t", k=2), "pl_u%d" % par, w=[("ua", par)])
            dma("sp", ua[:, :, 16:], uT[2 * g:2 * g + 2, :, 0:TH].rearrange("k p t -> p k t"), "pl_u%d" % par, w=[("ua", par)])
        load_group(0)
        for g in range(4):
            par = g % 2
            ua, ub, uc, dl = uas[par], ubs[par], ucs[par], dls[par]
            uak, dlk = ("ua", par), ("dl", par)
            if g + 1 < 4:
                load_group(g + 1)
            P.add("dve", lambda e, ua=ua: e.tensor_scalar_mul(out=ua[:, :, 0:16], in0=ua[:, :, 0:16], scalar1=hflag[:, 0:1]), r=[uak, "hflag"], w=[uak])
            src, skey = ua, uak
            bufs = [(ub, ("ub", par)), (uc, ("uc", par))]
            step = 1
            for lev in range(g + 1):
                dstb, dkey = bufs[lev % 2]
                P.add("dve", lambda e, src=src, dstb=dstb, step=step: e.tensor_tensor(
                    out=dstb[:, :, 16:], in0=src[:, :, 16:], in1=src[:, :, 16 - step:16 + TH - step], op=ALU.add), r=[skey], w=[dkey])
                if lev < g:
                    P.add("pool", lambda e, src=src, dstb=dstb, step=step: e.tensor_tensor(
                        out=dstb[:, :, step:16], in0=src[:, :, step:16], in1=src[:, :, 0:16 - step], op=ALU.add), r=[skey], w=[dkey])
                src, skey = dstb, dkey
                step *= 2
            wlen = 2 ** (g + 1)
            P.add("dve", lambda e, src=src, g=g: e.tensor_tensor(
                out=src[:, :, 16:32], in0=src[:, :, 16:32], in1=corr[:, g, :].unsqueeze(1).to_broadcast([128, 2, 16]), op=ALU.mult),
                r=[skey, "corr"], w=[skey])
            P.add("dve", lambda e, src=src, wlen=wlen, ua=ua, dl=dl: e.scalar_tensor_tensor(
                out=dl[:], in0=src[:, :, 16:], scalar=1.0 / wlen, in1=ua[:, :, 16:], op0=ALU.mult, op1=ALU.subtract), r=[skey, uak], w=[dlk])
            for dblk in range(2):
                si = nst % 2
                nst += 1
                for tb in range(4):
                    b = bank()

                    def mm(e, g=g, dblk=dblk, tb=tb, b=b, dl=dl):
                        ins = None
                        for c in range(2):
                            ins = e.matmul(ps[:, b, :], lhsT=pw[:, g, c, dblk * 128:(dblk + 1) * 128], rhs=dl[:, c, tb * 512:(tb + 1) * 512], start=(c == 0), stop=(c == 1))
                        return ins
                    P.add("pe", mm, r=[dlk, ("pw", g)], w=[PS(b)])
                    ch = g * 2 + dblk
                    P.add("act", lambda e, si=si, tb=tb, b=b, ch=ch: e.activation(
                        out=stb[si][:, tb * 512:(tb + 1) * 512], in_=ps[:, b, :], func=AF.Copy, scale=pscale[:, l, ch:ch + 1]), r=[PS(b)], w=[("stb", si, tb)])
                dma("sp", yT[g * 2 + dblk, :, h0:h0 + TH], stb[si][:], "pl_o%d" % si, r=[("stb", si, tb) for tb in range(4)])
        P.barrier()

    def phase_diff(hf, l):
        sb.off = PH0
        gk, gv = gk_all[l % 2], gv_all[l % 2]
        wb = sb.alloc([128, 8, 1024], F32)
        wbh = sb.alloc([128, 8, 512], F32)
        nk = 2 * TH
        kT = [sb.alloc([128, nk], BF16) for _ in range(2)]
        vv = [sb.alloc([128, nk // 128, 128], BF16) for _ in range(2)]
        qA = [sb.alloc([128, TH], BF16) for _ in range(2)]
        qB = [sb.alloc([128, TH], BF16) for _ in range(2)]
        NE, LA = 3, 2
        et = [[sb.alloc([128, 512], BF16) for _ in range(NE)] for _ in range(2)]
        tmp = [sb.alloc([128, 512], F32) for _ in range(2)]
        ocp = [[sb.alloc([128, 512], F32) for _ in range(4)] for _ in range(2)]
        sqs = [sb.alloc([128, 512], BF16) for _ in range(2)]
        rss = [sb.alloc([128, 512], F32) for _ in range(2)]
        yst = [sb.alloc([128, 512], BF16) for _ in range(2)]
        h0 = 0
        pending = [None]
        blk_cnt = [0]
        dma("sp", wb[:].rearrange("p a b -> p (a b)"), c_wbias[:, :], "df_wb", w=["wb"])
        dma("sp", wbh[:].rearrange("p a b -> p (a b)"), c_wbiash[:, :], "df_wb", w=["wbh"])
        for s_ in range(2):
            P.add("pool", lambda e, s_=s_: e.memset(qA[s_][64:128, :], 0.0), w=[("qA", s_)])
            P.add("pool", lambda e, s_=s_: e.memset(qB[s_][0:64, :], 0.0), w=[("qB", s_)])
        B_S = [0, 1, 2, 3]
        B_O = [4, 5]
        B_R = [6, 7]
        scnt = 0
        ycnt = 0
        def load_head(h):
            s_ = h % 2
            dma("sp", kT[s_][:, 0:TH], gk[h // 4][0:1024, :].rearrange("(k p x) t -> k p (x t)", k=4, p=128, x=2)[h % 4], "df_k%d" % s_, w=[("kT", s_)])
            dma("sp", kT[s_][:, TH:2 * TH], kdT[h, :, :], "df_k%d" % s_, w=[("kT", s_)])
            for j in range(2):
                dma("sp", vv[s_][:, j * 8:(j + 1) * 8, :], gv[j][0:1024, h * 128:(h + 1) * 128].rearrange("(c p) d -> p c d", p=128), "df_v%d" % s_, w=[("vv", s_)])
            dma("sp", vv[s_][:, 16:32, :], vd[:, h * 128:(h + 1) * 128].rearrange("(c p) d -> p c d", p=128), "df_v%d" % s_, w=[("vv", s_)])
            dma("sp", qA[s_][0:64, :], qdT[h, 0:64, h0:h0 + TH], "df_q%d" % s_, w=[("qA", s_)])
            dma("sp", qB[s_][64:128, :], qdT[h, 64:128, h0:h0 + TH], "df_q%d" % s_, w=[("qB", s_)])
        load_head(0)
        for h in range(8):
            s_ = h % 2
            if h + 1 < 8:
                load_head(h + 1)
            for qb in range(4):
                q0 = TH + qb * 512
                nkc = (q0 + 512) // 128

                def issue_S(kc, qb=qb, q0=q0, h=h, s_=s_):
                    r_ = q0 - kc * 128
                    ei = kc % NE
                    for m in range(2):
                        b = B_S[(kc % 2) * 2 + m]
                        qq = qA if m == 0 else qB
                        P.add("pe", lambda e, b=b, qq=qq, kc=kc: e.matmul(
                            ps[:, b, :], lhsT=kT[s_][:, kc * 128:(kc + 1) * 128], rhs=qq[s_][:, qb * 512:(qb + 1) * 512], start=True, stop=True),
                            r=[("kT", s_), ("qA" if m == 0 else "qB", s_)], w=[PS(b)])
                        ek = ("et", m, ei)
                        hist = kc < 16
                        if r_ >= 256:
                            bt = bfarh if hist else bfar
                            P.add("act", lambda e, b=b, m=m, ei=ei, bt=bt: e.activation(
                                out=et[m][ei][:], in_=ps[:, b, :], func=AF.Exp, scale=0.125, bias=bt[:, h:h + 1]), r=[PS(b)], w=[ek])
                        else:
                            c0 = r_ + 384
                            if hist:
                                assert c0 == 512
                                tab = wbh[:, h, 0:512]
                            else:
                                tab = wb[:, h, c0:c0 + 512]
                            P.add("dve", lambda e, b=b, m=m, tab=tab: e.scalar_tensor_tensor(
                                out=tmp[m][:], in0=ps[:, b, :], scalar=0.125, in1=tab, op0=ALU.mult, op1=ALU.add),
                                r=[PS(b), "wb", "wbh"], w=[("tmp", m)])
                            P.add("act", lambda e, m=m, ei=ei: e.activation(out=et[m][ei][:], in_=tmp[m][:], func=AF.Exp), r=[("tmp", m)], w=[ek])

                def issue_PV(kc, nkc=nkc, h=h, s_=s_):
                    ei = kc % NE
                    for m in range(2):
                        ek = ("et", m, ei)
                        P.add("pe", lambda e, m=m, ei=ei, kc=kc: e.matmul(
                            ps[:, B_O[m], :], lhsT=vv[s_][:, kc, :], rhs=et[m][ei][:], start=(kc == 0), stop=(kc == nkc - 1)),
                            r=[ek, ("vv", s_)], w=[PS(B_O[m])])
                        P.add("pe", lambda e, m=m, ei=ei, kc=kc: e.matmul(
                            ps[:, B_R[m], :], lhsT=onesb[:], rhs=et[m][ei][:], start=(kc == 0), stop=(kc == nkc - 1)),
                            r=[ek], w=[PS(B_R[m])])
                for i in range(nkc + LA):
                    if i < nkc:
                        issue_S(i)
                    if i >= LA:
                        issue_PV(i - LA)
                    if i == 5 and pending[0] is not None:
                        pending[0]()
                        pending[0] = None
                par = blk_cnt[0] % 2
                blk_cnt[0] += 1
                o1c, o2c, r1c, r2c = ocp[par]
                P.add("act", lambda e, o1c=o1c: e.activation(out=o1c[:], in_=ps[:, B_O[0], :], func=AF.Copy), r=[PS(B_O[0])], w=[("oc", par, 0)])
                P.add("dve", lambda e, r1c=r1c: e.tensor_copy(out=r1c[:], in_=ps[:, B_R[0], :]), r=[PS(B_R[0])], w=[("oc", par, 2)])
                P.add("act", lambda e, o2c=o2c: e.activation(out=o2c[:], in_=ps[:, B_O[1], :], func=AF.Copy), r=[PS(B_O[1])], w=[("oc", par, 1)])
                P.add("dve", lambda e, r2c=r2c: e.tensor_copy(out=r2c[:], in_=ps[:, B_R[1], :]), r=[PS(B_R[1])], w=[("oc", par, 3)])

                def part_b(h=h, qb=qb, par=par, o1c=o1c, o2c=o2c, r1c=r1c, r2c=r2c):
                    sq, rs = sqs[par], rss[par]
                    P.add("dve", lambda e: e.reciprocal(out=r1c[:], in_=r1c[:]), r=[("oc", par, 2)], w=[("oc", par, 2)])
                    P.add("dve", lambda e: e.reciprocal(out=r2c[:], in_=r2c[:]), r=[("oc", par, 3)], w=[("oc", par, 3)])
                    P.add("dve", lambda e: e.tensor_tensor(out=o1c[:], in0=o1c[:], in1=r1c[:], op=ALU.mult), r=[("oc", par, 0), ("oc", par, 2)], w=[("oc", par, 0)])
                    P.add("pool", lambda e: e.tensor_tensor(out=o2c[:], in0=o2c[:], in1=r2c[:], op=ALU.mult), r=[("oc", par, 1), ("oc", par, 3)], w=[("oc", par, 1)])
                    P.add("dve", lambda e: e.scalar_tensor_tensor(out=o1c[:], in0=o2c[:], scalar=neglam[:, l:l + 1], in1=o1c[:], op0=ALU.mult, op1=ALU.add),
                          r=[("oc", par, 0), ("oc", par, 1), "neglam"], w=[("oc", par, 0)])
                    P.add("act", lambda e: e.activation(out=sq[:], in_=o1c[:], func=AF.Square), r=[("oc", par, 0)], w=[("sq", par)])
                    b = B_S[0]
                    P.add("pe", lambda e: e.matmul(ps[:, b, :], lhsT=onesb[:], rhs=sq[:], start=True, stop=True), r=[("sq", par)], w=[PS(b)])
                    P.add("act", lambda e: e.activation(out=rs[:], in_=ps[:, b, :], func=AF.Sqrt, scale=1.0 / 128, bias=epsc[:]), r=[PS(b)], w=[("rs", par)])
                    P.add("dve", lambda e: e.reciprocal(out=rs[:], in_=rs[:]), r=[("rs", par)], w=[("rs", par)])
                    P.add("dve", lambda e: e.tensor_scalar(out=rs[:], in0=rs[:], scalar1=hgain[:, l:l + 1], scalar2=1.0 - lambda_init(l), op0=ALU.mult, op1=ALU.mult),
                          r=[("rs", par), "hgain"], w=[("rs", par)])
                    P.add("pool", lambda e: e.tensor_tensor(out=yst[par][:], in0=o1c[:], in1=rs[:], op=ALU.mult), r=[("oc", par, 0), ("rs", par)], w=[("yst", par)])
                    dma("sp", yT[8 + h, :, qb * 512:(qb + 1) * 512], yst[par][:], "df_o%d" % par, r=[("yst", par)])
                pending[0] = part_b
        if pending[0] is not None:
            pending[0]()
            pending[0] = None
        P.barrier()

    def phase_ret(hf, l, state_only=False):
        sb.off = PH0
        gst = gst_all[l % 2]
        cosb = sb.alloc([128, TH], F32)
        sinb = sb.alloc([128, TH], F32)
        dec = sb.alloc([128, 8, 2, 128], F32)
        mask = sb.alloc([128, 512], F32)
        raw = [[sb.alloc([128, TH], BF16) for _ in range(2)] for _ in range(2)]
        vt = [sb.alloc([128, 16, 128], BF16) for _ in range(2)]
        gt = [sb.alloc([128, TH], BF16) for _ in range(2)]
        rot = [sb.alloc([128, TH], BF16) for _ in range(2)]
        ktok = sb.alloc([128, 16, 128], BF16)
        stbf = sb.alloc([128, 16, 128], BF16)
        st32s = [sb.alloc([128, 128], F32) for _ in range(2)]
        PA = sb.alloc([128, 16, 128], F32)
        PB = sb.alloc([128, 16, 128], F32)
        Ug = sb.alloc([128, 16, 128], F32)
        t1 = [sb.alloc([128, 512], F32) for _ in range(2)]
        t2 = [sb.alloc([128, 512], F32) for _ in range(2)]
        mt = [sb.alloc([128, 512], BF16) for _ in range(2)]
        sqs = [sb.alloc([128, 512], BF16) for _ in range(2)]
        rss = [sb.alloc([128, 512], F32) for _ in range(2)]
        o32s = [sb.alloc([128, 512], F32) for _ in range(2)]
        yst = [sb.alloc([128, 512], BF16) for _ in range(2)]
        h0 = hf * TH
        dma("sp", cosb[:], c_cos[:, h0:h0 + TH], "rt_c", w=["cos"])
        dma("sp", sinb[:], c_sin[:, h0:h0 + TH], "rt_c", w=["sin"])
        dma("sp", dec[:].rearrange("p a b c -> p (a b c)"), c_dec[:, :], "rt_c", w=["dec"])
        dma("sp", mask[:], c_mask[:, :], "rt_c", w=["mask"])
        gam = [1.0 - 2.0 ** (-5.0 - h) for h in range(8)]
        tcnt = 0
        ycnt = 0
        def load_head(h):
            s_ = h % 2
            if not state_only:
                dma("sp", raw[s_][0][:], qrT[h, :, h0:h0 + TH], "rt_q%d" % s_, w=[("raw", s_, 0)])
                dma("sp", gt[s_][:], grT[h, :, h0:h0 + TH], "rt_g%d" % s_, w=[("gt", s_)])
                dma("sp", st32s[s_][:], gst[h * 128:(h + 1) * 128, :], "rt_s%d" % s_, w=[("st32", s_)])
            dma("sp", raw[s_][1][:], krT[h, :, h0:h0 + TH], "rt_k%d" % s_, w=[("raw", s_, 1)])
            dma("sp", vt[s_][:], vr[h0:h0 + TH, h * 128:(h + 1) * 128].rearrange("(c p) d -> p c d", p=128), "rt_v%d" % s_, w=[("vt", s_)])
        load_head(0)
        for h in range(8):
            s_ = h % 2
            st32 = st32s[s_]
            if h + 1 < 8:
                load_head(h + 1)
            if not state_only:
                P.add("dve", lambda e, st32=st32: e.tensor_scalar_mul(out=st32[:], in0=st32[:], scalar1=hflag[:, 0:1]), r=[("st32", s_), "hflag"], w=[("st32", s_)])
            for t in ((1,) if state_only else (0, 1)):
                for tb in range(4):
                    b = bank()
                    ti = tcnt % 2
                    tcnt += 1
                    sl = slice(tb * 512, (tb + 1) * 512)
                    P.add("pe", lambda e, b=b, s_=s_, t=t, sl=sl: e.matmul(ps[:, b, :], lhsT=swapb[:], rhs=raw[s_][t][:, sl], start=True, stop=True),
                          r=[("raw", s_, t)], w=[PS(b)])
                    P.add("pool", lambda e, ti=ti, s_=s_, t=t, sl=sl: e.tensor_tensor(out=t1[ti][:], in0=raw[s_][t][:, sl], in1=cosb[:, sl], op=ALU.mult),
                          r=[("raw", s_, t), "cos"], w=[("t1", ti)])
                    P.add("dve", lambda e, ti=ti, b=b, sl=sl: e.tensor_tensor(out=t2[ti][:], in0=ps[:, b, :], in1=sinb[:, sl], op=ALU.mult),
                          r=[PS(b), "sin"], w=[("t2", ti)])
                    P.add("pool", lambda e, ti=ti: e.tensor_tensor(out=t1[ti][:], in0=t1[ti][:], in1=t2[ti][:], op=ALU.add),
                          r=[("t1", ti), ("t2", ti)], w=[("t1", ti)])
                    P.add("dve", lambda e, ti=ti, t=t, sl=sl, h=h: e.tensor_tensor(
                        out=rot[t][:, sl].rearrange("p (a b) -> p a b", a=4), in0=t1[ti][:].rearrange("p (a b) -> p a b", a=4),
                        in1=dec[:, h, t, :].unsqueeze(1).to_broadcast([128, 4, 128]), op=ALU.mult),
                        r=[("t1", ti), "dec"], w=[("rot", t, tb)])
            rotk = [("rot", 1, tb) for tb in range(4)]
            rotq = [("rot", 0, tb) for tb in range(4)]
            for cg in range(4):
                b = bank()

                def tr(e, b=b, cg=cg):
                    ins = None
                    pb = ps[:, b, :].bitcast(BF16)
                    for j in range(4):
                        c = cg * 4 + j
                        ins = e.transpose(out=pb[:, j * 128:(j + 1) * 128], in_=rot[1][:, c * 128:(c + 1) * 128], identity=identb[:])
                    return ins
                P.add("pe", tr, r=[("rot", 1, cg)], w=[PS(b)])
                P.add("act", lambda e, b=b, cg=cg: e.activation(out=ktok[:, cg * 4:(cg + 1) * 4, :].rearrange("p a b -> p (a b)"),
                                                                in_=ps[:, b, :].bitcast(BF16)[:, 0:512], func=AF.Copy), r=[PS(b)], w=[("ktok", cg)])
            kb = []
            for cg in range(4):
                b = bank()
                kb.append(b)

                def mmk(e, b=b, cg=cg, s_=s_):
                    ins = None
                    for j in range(4):
                        c = cg * 4 + j
                        ins = e.matmul(ps[:, b, j * 128:(j + 1) * 128], lhsT=ktok[:, c, :], rhs=vt[s_][:, c, :], start=True, stop=True)
                    return ins
                P.add("pe", mmk, r=[("ktok", cg), ("vt", s_)], w=[PS(b)])
            g128 = gam[h] ** 128
            for cg in range(4):
                evac_copy(PA[:, cg * 4:(cg + 1) * 4, :].rearrange("p a b -> p (a b)"), ps[:, kb[cg], :], r=[PS(kb[cg])], w=["PA"])
            src, skey, dstb, dkey = PA, "PA", PB, "PB"
            for sh in (1, 2, 4, 8):
                gs = g128 ** sh
                P.add("dve", lambda e, src=src, dstb=dstb, sh=sh, gs=gs: e.scalar_tensor_tensor(
                    out=dstb[:, sh:16, :], in0=src[:, 0:16 - sh, :], scalar=gs, in1=src[:, sh:16, :], op0=ALU.mult, op1=ALU.add), r=[skey], w=[dkey])
                P.add("pool", lambda e, src=src, dstb=dstb, sh=sh: e.tensor_copy(out=dstb[:, 0:sh, :], in_=src[:, 0:sh, :]), r=[skey], w=[dkey])
                src, skey, dstb, dkey = dstb, dkey, src, skey
            if state_only:
                P.add("dve", lambda e, src=src, st32=st32, g128=g128: e.tensor_scalar_mul(out=st32[:], in0=src[:, 15, :], scalar1=g128), r=[skey], w=[("st32", s_)])
                dma("sp", rst[h], st32[:], "rt_so", r=[("st32", s_)])
                continue
            for c in range(1, 16):
                P.add("dve", lambda e, c=c, st32=st32, g128=g128: e.tensor_scalar_mul(out=Ug[:, c, :], in0=st32[:], scalar1=g128 ** c), r=[("st32", s_)], w=[("Ug", c)])
            P.add("act", lambda e, st32=st32: e.activation(out=stbf[:, 0, :], in_=st32[:], func=AF.Copy), r=[("st32", s_)], w=["stbf0"])
            P.add("dve", lambda e, src=src, g128=g128: e.scalar_tensor_tensor(
                out=stbf[:, 1:16, :], in0=src[:, 0:15, :], scalar=g128, in1=Ug[:, 1:16, :], op0=ALU.mult, op1=ALU.add),
                r=[skey] + [("Ug", c) for c in range(1, 16)], w=["stbf"])
            for cg in range(4):
                b = bank()
                mi = cg % 2

                def mms(e, b=b, cg=cg):
                    ins = None
                    for j in range(4):
                        c = cg * 4 + j
                        ins = e.matmul(ps[:, b, j * 128:(j + 1) * 128], lhsT=rot[1][:, c * 128:(c + 1) * 128], rhs=rot[0][:, c * 128:(c + 1) * 128], start=True, stop=True)
                    return ins
                P.add("pe", mms, r=[("rot", 1, cg), ("rot", 0, cg)], w=[PS(b)])
                P.add("dve", lambda e, b=b, mi=mi: e.tensor_tensor(out=mt[mi][:], in0=ps[:, b, :], in1=mask[:], op=ALU.mult), r=[PS(b), "mask"], w=[("mt", mi)])
                bo = bank()

                def mmo(e, bo=bo, cg=cg, mi=mi, s_=s_):
                    ins = None
                    for j in range(4):
                        c = cg * 4 + j
                        e.matmul(ps[:, bo, j * 128:(j + 1) * 128], lhsT=vt[s_][:, c, :], rhs=mt[mi][:, j * 128:(j + 1) * 128], start=True, stop=False)
                        ins = e.matmul(ps[:, bo, j * 128:(j + 1) * 128], lhsT=stbf[:, c, :], rhs=rot[0][:, c * 128:(c + 1) * 128], start=False, stop=True)
                    return ins
                P.add("pe", mmo, r=[("mt", mi), ("vt", s_), "stbf", "stbf0", ("rot", 0, cg)], w=[PS(bo)])
                sq, rs, o32 = sqs[mi], rss[mi], o32s[mi]
                P.add("act", lambda e, bo=bo, sq=sq: e.activation(out=sq[:], in_=ps[:, bo, :], func=AF.Square), r=[PS(bo)], w=[("sq", mi)])
                bn = bank()
                P.add("pe", lambda e, bn=bn, sq=sq: e.matmul(ps[:, bn, :], lhsT=onesb[:], rhs=sq[:], start=True, stop=True), r=[("sq", mi)], w=[PS(bn)])
                P.add("act", lambda e, bn=bn, rs=rs: e.activation(out=rs[:], in_=ps[:, bn, :], func=AF.Sqrt, scale=1.0 / 128, bias=epsc[:]), r=[PS(bn)], w=[("rs", mi)])
                P.add("dve", lambda e, rs=rs: e.reciprocal(out=rs[:], in_=rs[:]), r=[("rs", mi)], w=[("rs", mi)])
                P.add("dve", lambda e, bo=bo, rs=rs, o32=o32: e.tensor_tensor(out=o32[:], in0=ps[:, bo, :], in1=rs[:], op=ALU.mult), r=[PS(bo), ("rs", mi)], w=[("o32", mi)])
                yi = ycnt % 2
                ycnt += 1
                P.add("pool", lambda e, yi=yi, s_=s_, cg=cg, o32=o32: e.tensor_tensor(out=yst[yi][:], in0=o32[:], in1=gt[s_][:, cg * 512:(cg + 1) * 512], op=ALU.mult),
                      r=[("o32", mi), ("gt", s_)], w=[("yst", yi)])
                t0 = h0 + cg * 512
                dma("sp", yT[16 + h, :, t0:t0 + 512], yst[yi][:], "rt_o%d" % yi, r=[("yst", yi)])
        P.barrier()

    def phase_merge(hf, l):
        sb.off = PH0
        yr = sb.alloc([128, 24, TH], BF16)
        wbuf = [sb.alloc([128, 24, 256], BF16) for _ in range(2)]
        gb = [sb.alloc([128, 3, 512], BF16) for _ in range(2)]
        m0 = [sb.alloc([128, 512], F32) for _ in range(2)]
        m1 = [sb.alloc([128, 512], F32) for _ in range(2)]
        m2 = [sb.alloc([128, 512], F32) for _ in range(2)]
        stb = [sb.alloc([128, TH], BF16) for _ in range(2)]
        h0 = hf * TH
        for n in range(3):
            dma("sp", yr[:, n * 8:(n + 1) * 8, :], yT[n * 8:(n + 1) * 8, :, h0:h0 + TH].rearrange("k p t -> p k t"), "mg_y%d" % n, w=[("yr", n)])
        gcnt = 0
        def ldw(wbk):
            slot = wbk % 2
            for n in range(3):
                dma("pool", wbuf[slot][:, n * 8:(n + 1) * 8, :], w_branch[l, n, :, wbk * 256:(wbk + 1) * 256].rearrange("(k p) c -> p k c", p=128),
                    "w%d" % slot, w=[("wbuf", slot, n)])
        ldw(0)
        for wbk in range(8):
            slot = wbk % 2
            if wbk + 1 < 8:
                ldw(wbk + 1)
            for sub in range(2):
                dblk = wbk * 2 + sub
                si = dblk % 2
                for tb in range(4):
                    gi = gcnt % 2
                    gcnt += 1
                    t0 = h0 + tb * 512
                    for n in range(3):
                        dma("sp", gb[gi][:, n, :], gaT[n * 16 + dblk, :, t0:t0 + 512], "mg_g%d" % gi, w=[("gb", gi, n)])
                    bs = []
                    for n in range(3):
                        b = bank()
                        bs.append(b)

                        def mm(e, b=b, n=n, slot=slot, sub=sub, tb=tb):
                            ins = None
                            for c in range(8):
                                ins = e.matmul(ps[:, b, :], lhsT=wbuf[slot][:, n * 8 + c, sub * 128:(sub + 1) * 128], rhs=yr[:, n * 8 + c, tb * 512:(tb + 1) * 512], start=(c == 0), stop=(c == 7))
                            return ins
                        P.add("pe", mm, r=[("wbuf", slot, n), ("yr", n)], w=[PS(b)])
                    mm_ = (m0[gi], m1[gi], m2[gi])
                    for n in range(3):
                        P.add("dve", lambda e, n=n, gi=gi, b=bs[n], mm_=mm_: e.tensor_tensor(out=mm_[n][:], in0=ps[:, b, :], in1=gb[gi][:, n, :], op=ALU.mult),
                              r=[PS(bs[n]), ("gb", gi, n)], w=[("m", gi, n)])
                    P.add("pool", lambda e, mm_=mm_: e.tensor_tensor(out=mm_[0][:], in0=mm_[0][:], in1=mm_[1][:], op=ALU.add),
                          r=[("m", gi, 0), ("m", gi, 1)], w=[("m", gi, 0)])
                    P.add("pool", lambda e, mm_=mm_, si=si, tb=tb: e.tensor_tensor(out=stb[si][:, tb * 512:(tb + 1) * 512], in0=mm_[0][:], in1=mm_[2][:], op=ALU.add),
                          r=[("m", gi, 0), ("m", gi, 2)], w=[("stb", si, tb)])
                dma("sp", mgT[dblk, :, h0:h0 + TH], stb[si][:], "mg_o%d" % si, r=[("stb", si, tb) for tb in range(4)])
        P.barrier()

    def proj_fm(hf, l, src_res, src_keys, nk, wsrc, ncol, dst, evac, dst_t0, ntb, tb_off=0, dt_=F32, wnk=None):
        wbuf = [sb.alloc([128, nk, 512], BF16) for _ in range(2)]
        st = [sb.alloc([128, ntb * 512], dt_) for _ in range(2)]
        nst = 0
        for cb in range(ncol // 512):
            slot = cb % 2
            load_w(wbuf, slot, wsrc[:, cb * 512:(cb + 1) * 512])
            for sub in range(4):
                si = nst % 2
                nst += 1
                for tb in range(ntb):
                    b = bank()

                    def mm(e, b=b, slot=slot, sub=sub, tb=tb):
                        ins = None
                        for k in range(nk):
                            ins = e.matmul(ps[:, b, :], lhsT=wbuf[slot][:, k, sub * 128:(sub + 1) * 128], rhs=src_res[:, k, tb * 512:(tb + 1) * 512], start=(k == 0), stop=(k == nk - 1))
                        return ins
                    P.add("pe", mm, r=[("wbuf", slot)] + src_keys(tb), w=[PS(b)])
                    evac(st[si][:, tb * 512:(tb + 1) * 512], b, ("pst", si, tb))
                dma("sp", dst[cb * 4 + sub, :, dst_t0:dst_t0 + ntb * 512], st[si][:], "pj_o%d" % si, r=[("pst", si, tb) for tb in range(ntb)])

    def phase_outproj(hf, l):
        sb.off = PH0
        mr = sb.alloc([128, 16, TH], BF16)
        h0 = hf * TH
        dma("sp", mr[:], mgT[:, :, h0:h0 + TH].rearrange("k p t -> p k t"), "op_m", w=["mr"])

        def evac(o, b, key):
            evac_copy(o, ps[:, b, :], r=[PS(b)], w=[key])
        proj_fm(hf, l, mr, lambda tb: ["mr"], 16, w_out[l], D, mixT, evac, h0, 4)
        P.barrier()

    def phase_mlp(hf, l):
        sb.off = PH0
        hT = sb.alloc([128, 16, TH], BF16)
        phase_prenorm(hf, l, 2, hT, "hT")
        h0 = hf * TH
        mark = sb.off
        sqt = [sb.alloc([128, 512], F32) for _ in range(2)]
        qn = [0]

        def evac(o, b, key):
            qi = qn[0] % 2
            qn[0] += 1
            P.add("act", lambda e, b=b, qi=qi: e.activation(out=sqt[qi][:], in_=ps[:, b, :], func=AF.Square), r=[PS(b)], w=[("sqt", qi)])
            P.add("dve", lambda e, b=b, qi=qi: e.scalar_tensor_tensor(out=o, in0=ps[:, b, :], scalar=0.0, in1=sqt[qi][:], op0=ALU.is_gt, op1=ALU.mult),
                  r=[PS(b), ("sqt", qi)], w=[key])
        proj_fm(hf, l, hT, lambda tb: [("hT", k, tb) for k in range(16)], 16, w_up[l], DFF, ffT, evac, 0, 4, dt_=BF16)
        P.barrier()
        sb.off = PH0
        fr = sb.alloc([128, 64, 1024], BF16)
        wbuf = [sb.alloc([128, 16, 512], BF16) for _ in range(2)]
        st = [sb.alloc([128, 1024], F32) for _ in range(2)]
        wn = 0
        nst = 0
        for tb2 in range(2):
            for kq in range(4):
                dma("sp", fr[:, kq * 16:(kq + 1) * 16, :], ffT[kq * 16:(kq + 1) * 16, :, tb2 * 1024:(tb2 + 1) * 1024].rearrange("k p t -> p k t"),
                    "dn_f%d" % kq, w=[("fr", kq)])
            for eb in range(4):
                for kq in range(4):
                    slot = wn % 2
                    wn += 1
                    load_w(wbuf, slot, w_down[l, kq * 2048:(kq + 1) * 2048, eb * 512:(eb + 1) * 512])
                    for sub in range(4):
                        for t in range(2):
                            b = sub * 2 + t

                            def mm(e, b=b, slot=slot, sub=sub, t=t, kq=kq):
                                ins = None
                                for k in range(16):
                                    ins = e.matmul(ps[:, b, :], lhsT=wbuf[slot][:, k, sub * 128:(sub + 1) * 128], rhs=fr[:, kq * 16 + k, t * 512:(t + 1) * 512],
                                                   start=(kq == 0 and k == 0), stop=(kq == 3 and k == 15))
                                return ins
                            P.add("pe", mm, r=[("wbuf", slot), ("fr", kq)], w=[PS(b)])
                for sub in range(4):
                    si = nst % 2
                    nst += 1
                    for t in range(2):
                        b = sub * 2 + t
                        evac_copy(st[si][:, t * 512:(t + 1) * 512], ps[:, b, :], r=[PS(b)], w=[("dst", si, t)])
                    t0 = h0 + tb2 * 1024
                    dma("sp", mixT[eb * 4 + sub, :, t0:t0 + 1024], st[si][:], "dn_o%d" % si, r=[("dst", si, 0), ("dst", si, 1)])
        P.barrier()

    PAIRS = [[0, 1], [2, 3], [4, 5], [6, 7]]

    def phase_exchange(l):
        gk, gv, gst, gtl = gk_all[l % 2], gv_all[l % 2], gst_all[l % 2], gtl_all[l % 2]
        dma("sp", utl.rearrange("p (k t) -> p k t", k=8), uT[:, :, TH - 16:TH].rearrange("k p t -> p k t"), "ex_t")
        P.barrier()

        def cc(i, src, dst):
            P.add("pool", lambda e: e.collective_compute("AllGather", ALU.bypass, replica_groups=PAIRS, ins=[src], outs=[dst]),
                  chan="cc%d" % i, inc=1)
        for j in range(2):
            cc(j, kdT[4 * j:4 * j + 4].rearrange("k p (x t) -> (k p x) t", x=2), gk[j][:, :])
            cc(2 + j, vd[j * 1024:(j + 1) * 1024, :], gv[j][:, :])
        cc(4, rst.rearrange("h p e -> (h p) e"), gst[:, :])
        cc(5, utl[:, :], gtl[:, :])
        P.barrier()

    phase_transpose_in()
    for l in range(n_layers):
        phase_inproj(0, l)
        phase_ret(0, l, state_only=True)
        phase_exchange(l)
        phase_pool(0, l)
        phase_diff(0, l)
        phase_ret(0, l)
        phase_merge(0, l)
        phase_outproj(0, l)
        phase_postnorm(0, l, 1)
        phase_mlp(0, l)
        phase_postnorm(0, l, 3)
    phase_transpose_out()
    P.add("sp", None, r=["OUT"])

    P.lower()
    chans = sorted(P.chan_cnt.keys())
    csem = {c: stack.enter_context(nc.semaphore("sc_" + c)) for c in chans}
    block = stack.enter_context(nc.Block())
    P.emit(nc, block, esem, csem)
    stack.close()
    return nc


def t5_bucket(n):
    n = np.maximum(n, 0)
    exact = 16
    nf = np.maximum(n, 1).astype(np.float32)
    large = exact + (np.log(nf / np.float32(exact)) / np.float32(math.log(128 / exact)) * np.float32(32 - exact)).astype(np.int32)
    large = np.minimum(large, 31)
    return np.where(n < exact, n, large)


def make_consts(inputs, hf):
    f32 = np.float32
    c = {}
    g = np.stack([inputs["norm_pre_mix"], inputs["norm_post_mix"], inputs["norm_pre_mlp"], inputs["norm_post_mlp"]], 0)
    c["c_gains"] = np.ascontiguousarray(g.reshape(4, DEPTH, 16, 128).transpose(3, 0, 1, 2).reshape(128, -1)).astype(f32)
    c["c_pscale"] = np.ascontiguousarray(inputs["pool_scale"].reshape(DEPTH, 8, 128).transpose(2, 0, 1).reshape(128, -1)).astype(f32)
    c["c_hgain"] = np.ascontiguousarray(inputs["diff_head_norm"].T).astype(f32)
    c["c_lam"] = np.ascontiguousarray(np.broadcast_to(inputs["diff_lambda"].reshape(1, -1), (128, DEPTH * 256))).astype(f32)
    rb = inputs["rel_bias"].astype(f32)
    c["c_bfar"] = np.ascontiguousarray(np.broadcast_to(rb[31:32, :], (128, 8))).astype(f32)
    p = np.arange(128)[:, None]
    cc = np.arange(1024)[None, :]
    dist = cc - 384 - p
    bk = t5_bucket(dist)
    wb = rb[bk, :]
    wb = np.where((dist >= 0)[:, :, None], wb, f32(-1e30))
    wbt = np.ascontiguousarray(wb.transpose(0, 2, 1)).astype(f32)
    c["c_wbias"] = wbt.reshape(128, -1)
    if hf == 1:
        c["c_wbiash"] = np.ascontiguousarray(wbt[:, :, 512:1024]).reshape(128, -1)
        c["c_bfarh"] = c["c_bfar"].copy()
    else:
        c["c_wbiash"] = np.full((128, 8 * 512), -1e30, f32)
        c["c_bfarh"] = np.full((128, 8), -1e30, f32)
    c["c_hflag"] = np.full((128, 1), float(hf), f32)
    half = 64
    inv_freq = (f32(1.0) / (f32(10000.0) ** np.linspace(0.0, 1.0, half, dtype=f32))).astype(f32)
    ang = (np.arange(S, dtype=f32)[:, None] * inv_freq[None, :]).astype(f32)
    cos = np.cos(ang).astype(f32).T
    sin = np.sin(ang).astype(f32).T
    c["c_cos"] = np.ascontiguousarray(np.concatenate([cos, cos], 0)[:, hf * TH:(hf + 1) * TH])
    c["c_sin"] = np.ascontiguousarray(np.concatenate([-sin, sin], 0)[:, hf * TH:(hf + 1) * TH])
    hh = np.arange(8, dtype=np.float64)
    lg = np.log(1.0 - 2.0 ** (-5.0 - hh))
    i = np.arange(128, dtype=np.float64)
    qd = np.exp((i[None, :] + 1) * lg[:, None])
    kd = np.exp(-(i[None, :] + 1) * lg[:, None]) * (128 ** -0.5)
    dec = np.stack([qd, kd], 1)
    c["c_dec"] = np.ascontiguousarray(np.broadcast_to(dec.reshape(1, -1), (128, 8 * 2 * 128))).astype(f32)
    corr = np.zeros((4, 16), np.float64)
    for gi in range(4):
        w = 2 ** (gi + 1)
        for t in range(16):
            corr[gi, t] = (w / min(t + 1, w)) if hf == 0 else 1.0
    c["c_corr"] = np.ascontiguousarray(np.broadcast_to(corr.reshape(1, -1), (128, 64))).astype(f32)
    j = np.arange(128)[:, None]
    ii = np.arange(512)[None, :] % 128
    c["c_mask"] = (ii >= j).astype(f32)
    c["c_ident"] = np.eye(128, dtype=f32)
    sw = np.zeros((128, 128), f32)
    for m in range(128):
        sw[(m + 64) % 128, m] = 1.0
    c["c_swap"] = sw
    return c


_NC_CACHE = {}


def kernel(**inputs):
    inputs = {k: np.asarray(v) for k, v in inputs.items()}
    if "nc" not in _NC_CACHE:
        _NC_CACHE["nc"] = build_program()
    nc = _NC_CACHE["nc"]
    consts = [make_consts(inputs, 0), make_consts(inputs, 1)]
    shared = {k: np.ascontiguousarray(inputs[k], dtype=np.float32) for k in ("w_in", "pool_w", "w_branch", "w_out", "w_up", "w_down")}
    in_maps = []
    for core in range(8):
        b, hf = core // 2, core % 2
        m = dict(shared)
        m.update(consts[hf])
        m["x"] = np.ascontiguousarray(inputs["x"][b, hf * TH:(hf + 1) * TH], dtype=np.float32)
        in_maps.append(m)
    res = run_bass_kernel_spmd(nc, in_maps, core_ids=list(range(8)))
    full = np.empty((4, S, D), np.float32)
    for core in range(8):
        b, hf = core // 2, core % 2
        full[b, hf * TH:(hf + 1) * TH] = np.asarray(res.results[core]["out"], dtype=np.float32)
    return full
```

```python
import math
import numpy as np
from contextlib import ExitStack
import concourse.bass as bass
import concourse.mybir as mybir
from concourse.bass_utils import run_bass_kernel_spmd

F32 = mybir.dt.float32
BF16 = mybir.dt.bfloat16
AF = mybir.ActivationFunctionType
ALU = mybir.AluOpType

D = 2048
S = 4096
TH = 2048
NHALF = 2
DEPTH = 4
MIX = 1024
DFF = 8192
INC = 14336
EPS = 1e-6
ENGS = ("pe", "act", "dve", "pool", "sp")


class Op:
    __slots__ = ("eng", "fn", "deps", "signal", "chan", "cidx", "sig", "waits", "know", "gid", "inc")


class Prog:
    def __init__(self):
        self.ops = {e: [] for e in ENGS}
        self.all = []
        self.last_w = {}
        self.readers = {}
        self.chan_last = {}
        self.chan_cnt = {}
        self.bar = {e: [] for e in ENGS}

    def add(self, eng, fn, r=(), w=(), chan=None, inc=16):
        op = Op()
        op.eng = eng
        op.fn = fn
        op.chan = chan
        op.signal = False
        op.cidx = 0
        deps = set()
        for k in r:
            lw = self.last_w.get(k)
            if lw is not None:
                deps.add(lw)
        for k in w:
            lw = self.last_w.get(k)
            if lw is not None:
                deps.add(lw)
            for rd in self.readers.get(k, ()):
                deps.add(rd)
        if chan is not None:
            cl = self.chan_last.get(chan)
            if cl is not None:
                deps.add(cl)
            self.chan_last[chan] = op
            op.cidx = self.chan_cnt.get(chan, 0) + inc
            self.chan_cnt[chan] = op.cidx
            op.inc = inc
        for d in self.bar[eng]:
            deps.add(d)
        self.bar[eng] = []
        deps.discard(op)
        if eng == "pe":
            deps = {d for d in deps if not (d.eng == "pe" and d.chan is None)}
        op.deps = deps
        for k in w:
            self.last_w[k] = op
            self.readers[k] = []
        for k in r:
            self.readers.setdefault(k, []).append(op)
        op.gid = len(self.all)
        self.all.append(op)
        self.ops[eng].append(op)
        return op

    def barrier(self):
        lst = []
        for e in ENGS:
            for op in reversed(self.ops[e]):
                if op.chan is None and op.fn is not None:
                    lst.append(op)
                    break
        for c, op in self.chan_last.items():
            lst.append(op)
        for e in ENGS:
            self.bar[e] = list(lst)

    def lower(self):
        for op in self.all:
            for d in op.deps:
                d.signal = True
        cnt = {e: 0 for e in ENGS}
        know = {e: {} for e in ENGS}
        for op in self.all:
            e = op.eng
            waits = {}
            kn = know[e]
            for d in op.deps:
                if d.chan is not None:
                    key, val = ("c", d.chan), d.cidx
                else:
                    key, val = ("e", d.eng), d.sig
                if kn.get(key, 0) >= val:
                    continue
                if waits.get(key, 0) < val:
                    waits[key] = val
                new = dict(kn)
                for k2, v2 in d.know.items():
                    if new.get(k2, 0) < v2:
                        new[k2] = v2
                kn = new
            know[e] = kn
            op.waits = list(waits.items())
            if op.chan is not None:
                kk = dict(kn)
                kk[("c", op.chan)] = op.cidx
                op.know = kk
                op.sig = 0
            elif op.signal:
                cnt[e] += 1
                op.sig = cnt[e]
                kk = dict(kn)
                kk[("e", e)] = op.sig
                op.know = kk
            else:
                op.sig = 0
                op.know = kn

    def emit(self, nc, block, esem, csem):
        def run(ename):
            def body(eng):
                for op in self.ops[ename]:
                    for key, val in op.waits:
                        sem = csem[key[1]] if key[0] == "c" else esem[key[1]]
                        eng.wait_ge(sem, val)
                    if op.fn is None:
                        continue
                    ins = op.fn(eng)
                    if op.chan is not None:
                        ins.then_inc(csem[op.chan], op.inc) if op.inc != 1 else ins.then_inc(csem[op.chan])
                    elif op.signal:
                        ins.then_inc(esem[ename], 1)
            return body
        block.tensor(run("pe"))
        block.scalar(run("act"))
        block.vector(run("dve"))
        block.gpsimd(run("pool"))
        block.sync(run("sp"))


class SBAlloc:
    def __init__(self, nc, base=20480, limit=204800):
        self.nc = nc
        self.off = base
        self.limit = limit
        self.n = 0

    def alloc(self, shape, dtype):
        nb = 4 if dtype == F32 else 2
        sz = nb
        for s_ in shape[1:]:
            sz *= s_
        sz = (sz + 63) // 64 * 64
        t = self.nc.alloc_sbuf_tensor_at("sb%d" % self.n, list(shape), dtype, offset=self.off)
        self.n += 1
        self.off += sz
        assert self.off <= self.limit, ("SBUF overflow", self.off)
        return t


def lambda_init(l):
    return 0.8 - 0.6 * math.exp(-0.3 * l)


def build_program(n_layers=DEPTH, debug=False):
    nc = bass.Bass("TRN2", target_bir_lowering=False)
    P = Prog()

    def din(name, shape, dt=F32):
        return nc.dram_tensor(name, list(shape), dt, kind="ExternalInput").ap()

    x_in = din("x", [TH, D])
    w_in = din("w_in", [DEPTH, D, INC])
    pool_w = din("pool_w", [DEPTH, 4, 256, 256])
    w_branch = din("w_branch", [DEPTH, 3, MIX, D])
    w_out = din("w_out", [DEPTH, D, D])
    w_up = din("w_up", [DEPTH, D, DFF])
    w_down = din("w_down", [DEPTH, DFF, D])
    c_gains = din("c_gains", [128, 4 * DEPTH * 16])
    c_pscale = din("c_pscale", [128, DEPTH * 8])
    c_hgain = din("c_hgain", [128, DEPTH])
    c_lam = din("c_lam", [128, DEPTH * 256])
    c_bfar = din("c_bfar", [128, 8])
    c_wbias = din("c_wbias", [128, 8 * 1024])
    c_cos = din("c_cos", [128, TH])
    c_sin = din("c_sin", [128, TH])
    c_dec = din("c_dec", [128, 8 * 2 * 128])
    c_corr = din("c_corr", [128, 4 * 16])
    c_mask = din("c_mask", [128, 512])
    c_ident = din("c_ident", [128, 128])
    c_swap = din("c_swap", [128, 128])
    c_hflag = din("c_hflag", [128, 1])
    c_bfarh = din("c_bfarh", [128, 8])
    c_wbiash = din("c_wbiash", [128, 8 * 512])
    out = nc.dram_tensor("out", [TH, D], F32, kind="ExternalOutput").ap()

    def dscr(name, shape, dt):
        kind = "ExternalOutput" if (debug and name in ("s_yT", "s_xT")) else "Internal"
        return nc.dram_tensor(name, list(shape), dt, kind=kind).ap()

    xT = dscr("s_xT", [16, 128, TH], F32)
    mixT = dscr("s_mixT", [16, 128, TH], F32)
    uT = dscr("s_uT", [8, 128, TH], F32)
    qdT = dscr("s_qdT", [8, 128, TH], BF16)
    kdT = dscr("s_kdT", [8, 128, TH], BF16)
    vd = dscr("s_vd", [TH, MIX], BF16)
    qrT = dscr("s_qrT", [8, 128, TH], BF16)
    krT = dscr("s_krT", [8, 128, TH], BF16)
    vr = dscr("s_vr", [TH, MIX], BF16)
    grT = dscr("s_grT", [8, 128, TH], BF16)
    gaT = dscr("s_gaT", [48, 128, TH], BF16)
    yT = dscr("s_yT", [24, 128, TH], BF16)
    mgT = dscr("s_mgT", [16, 128, TH], BF16)
    ffT = dscr("s_ffT", [64, 128, TH], BF16)
    rst = dscr("s_rst", [8, 128, 128], F32)
    utl = dscr("s_utl", [128, 128], F32)
    gk_all = [[dscr("g_k%d_%d" % (j, par), [2048, 1024], BF16) for j in range(2)] for par in range(2)]
    gv_all = [[dscr("g_v%d_%d" % (j, par), [2048, 1024], BF16) for j in range(2)] for par in range(2)]
    gst_all = [dscr("g_st_%d" % par, [2048, 128], F32) for par in range(2)]
    gtl_all = [dscr("g_tl_%d" % par, [256, 128], F32) for par in range(2)]

    sb = SBAlloc(nc)
    ident = sb.alloc([128, 128], F32)
    identb = sb.alloc([128, 128], BF16)
    swapb = sb.alloc([128, 128], BF16)
    onesb = sb.alloc([128, 128], BF16)
    gains = sb.alloc([128, 4, DEPTH, 16], F32)
    pscale = sb.alloc([128, DEPTH, 8], F32)
    hgain = sb.alloc([128, DEPTH], F32)
    lamt = sb.alloc([128, DEPTH, 4, 64], F32)
    bfar = sb.alloc([128, 8], F32)
    neglam = sb.alloc([128, DEPTH], F32)
    lamtmp = sb.alloc([128, 2, 64], F32)
    lamred = sb.alloc([128, 2], F32)
    epsc = sb.alloc([128, 1], F32)
    hflag = sb.alloc([128, 1], F32)
    bfarh = sb.alloc([128, 8], F32)
    PH0 = sb.off

    stack = ExitStack()
    ps = stack.enter_context(nc.psum_tensor("ps", [128, 8, 512], F32))
    esem = {e: stack.enter_context(nc.semaphore("se_" + e)) for e in ENGS}

    psn = [0]

    def bank():
        b = psn[0] % 8
        psn[0] += 1
        return b

    def PS(b):
        return ("ps", b)

    def dma(eng, out_ap, in_ap, chan, r=(), w=()):
        return P.add(eng, lambda e: e.dma_start(out=out_ap, in_=in_ap), r=r, w=w, chan=chan)

    ev_rr = [0]

    def evac_copy(out_ap, in_ap, r, w, eng=None):
        if eng is None:
            eng = ("act", "dve")[ev_rr[0] % 2]
            ev_rr[0] += 1
        if eng == "act":
            return P.add("act", lambda e: e.activation(out=out_ap, in_=in_ap, func=AF.Copy), r=r, w=w)
        return P.add(eng, lambda e: e.tensor_copy(out=out_ap, in_=in_ap), r=r, w=w)

    P.add("dve", lambda e: e.memset(onesb[:], 1.0), w=["onesb"])
    P.add("dve", lambda e: e.memset(epsc[:], EPS), w=["epsc"])
    dma("sp", ident[:], c_ident[:, :], "c0", w=["ident"])
    dma("pool", identb[:], c_ident[:, :], "c1", w=["identb"])
    dma("pool", swapb[:], c_swap[:, :], "c1", w=["swapb"])
    dma("sp", gains[:].rearrange("p a b c -> p (a b c)"), c_gains[:, :], "c0", w=["gains"])
    dma("sp", pscale[:].rearrange("p a b -> p (a b)"), c_pscale[:, :], "c0", w=["pscale"])
    dma("sp", hgain[:], c_hgain[:, :], "c0", w=["hgain"])
    dma("sp", lamt[:].rearrange("p a b c -> p (a b c)"), c_lam[:, :], "c0", w=["lamt"])
    dma("sp", bfar[:], c_bfar[:, :], "c0", w=["bfar"])
    dma("sp", bfarh[:], c_bfarh[:, :], "c0", w=["bfarh"])
    dma("sp", hflag[:], c_hflag[:, :], "c0", w=["hflag"])
    for l in range(n_layers):
        P.add("dve", lambda e, l=l: e.tensor_tensor(out=lamtmp[:, 0, :], in0=lamt[:, l, 0, :], in1=lamt[:, l, 1, :], op=ALU.mult), r=["lamt"], w=["lamtmp0"])
        P.add("dve", lambda e, l=l: e.tensor_tensor(out=lamtmp[:, 1, :], in0=lamt[:, l, 2, :], in1=lamt[:, l, 3, :], op=ALU.mult), r=["lamt"], w=["lamtmp1"])
        P.add("dve", lambda e: e.reduce_sum(out=lamred[:], in_=lamtmp[:], axis=mybir.AxisListType.X), r=["lamtmp0", "lamtmp1"], w=["lamred"])
        P.add("act", lambda e: e.activation(out=lamred[:], in_=lamred[:], func=AF.Exp), r=["lamred"], w=["lamred"])
        P.add("dve", lambda e, l=l: e.tensor_tensor(out=neglam[:, l:l + 1], in0=lamred[:, 1:2], in1=lamred[:, 0:1], op=ALU.subtract), r=["lamred"], w=["neglam"])
        P.add("dve", lambda e, l=l: e.tensor_scalar_add(out=neglam[:, l:l + 1], in0=neglam[:, l:l + 1], scalar1=-lambda_init(l)), r=["neglam"], w=["neglam"])
    P.barrier()

    def phase_transpose_in():
        sb.off = PH0
        xin = [sb.alloc([128, 4, D], F32) for _ in range(2)]
        stg = [sb.alloc([128, 16, 512], F32) for _ in range(2)]
        for tg in range(TH // 512):
            s_ = tg % 2
            dma("sp", xin[s_][:], x_in[tg * 512:(tg + 1) * 512, :].rearrange("(j p) d -> p j d", p=128), "ti_in%d" % s_, w=[("xin", s_)])
            for k in range(16):
                b = bank()

                def tr(e, k=k, b=b, s_=s_):
                    ins = None
                    for j in range(4):
                        ins = e.transpose(out=ps[:, b, j * 128:(j + 1) * 128], in_=xin[s_][:, j, k * 128:(k + 1) * 128], identity=ident[:])
                    return ins
                P.add("pe", tr, r=[("xin", s_)], w=[PS(b)])
                evac_copy(stg[s_][:, k, :], ps[:, b, :], r=[PS(b)], w=[("tstg", s_, k)])
            dma("sp", xT[:, :, tg * 512:(tg + 1) * 512].rearrange("k p t -> p k t"), stg[s_][:], "ti_out%d" % s_, r=[("tstg", s_, k) for k in range(16)])
        P.barrier()

    def phase_transpose_out():
        sb.off = PH0
        xin = [sb.alloc([128, 16, 512], F32) for _ in range(2)]
        stg = [sb.alloc([128, 4, D], F32) for _ in range(2)]
        for tg in range(TH // 512):
            s_ = tg % 2
            dma("sp", xin[s_][:], xT[:, :, tg * 512:(tg + 1) * 512].rearrange("k p t -> p k t"), "to_in%d" % s_, w=[("xin", s_)])
            for j in range(4):
                for kg in range(4):
                    b = bank()

                    def tr(e, j=j, kg=kg, b=b, s_=s_):
                        ins = None
                        for kk in range(4):
                            k = kg * 4 + kk
                            ins = e.transpose(out=ps[:, b, kk * 128:(kk + 1) * 128], in_=xin[s_][:, k, j * 128:(j + 1) * 128], identity=ident[:])
                        return ins
                    P.add("pe", tr, r=[("xin", s_)], w=[PS(b)])
                    evac_copy(stg[s_][:, j, kg * 512:(kg + 1) * 512], ps[:, b, :], r=[PS(b)], w=[("tstg", s_, j, kg)])
            dma("sp", out[tg * 512:(tg + 1) * 512, :].rearrange("(j p) d -> p j d", p=128), stg[s_][:], "to_out%d" % s_,
                r=[("tstg", s_, j, kg) for j in range(4) for kg in range(4)], w=["OUT"])
        P.barrier()

    def stats_block(blk, blk_key, sqb, rstd_ap, rstd_key, nd):
        b = bank()
        sqa, sqk = sqb
        P.add("act", lambda e: e.activation(out=sqa[:, 0:10, :], in_=blk[:, 0:10, :], func=AF.Square), r=[blk_key], w=[(sqk, 0)])
        P.add("pool", lambda e: e.tensor_tensor(out=sqa[:, 10:16, :], in0=blk[:, 10:16, :], in1=blk[:, 10:16, :], op=ALU.mult), r=[blk_key], w=[(sqk, 1)])

        def mmq(e, b=b):
            ins = None
            for k in range(16):
                ins = e.matmul(ps[:, b, :], lhsT=onesb[:], rhs=sqa[:, k, :], start=(k == 0), stop=(k == 15))
            return ins
        P.add("pe", mmq, r=[(sqk, 0), (sqk, 1)], w=[PS(b)])
        P.add("act", lambda e, b=b: e.activation(out=rstd_ap, in_=ps[:, b, :], func=AF.Sqrt, scale=1.0 / nd, bias=epsc[:]), r=[PS(b)], w=[rstd_key])
        P.add("dve", lambda e: e.reciprocal(out=rstd_ap, in_=rstd_ap), r=[rstd_key], w=[rstd_key])

    def phase_prenorm(hf, l, norm_idx, dst, dst_key):
        mark = sb.off
        xb = [sb.alloc([128, 16, 512], F32) for _ in range(2)]
        sqb = [sb.alloc([128, 16, 512], BF16) for _ in range(2)]
        rstd = [sb.alloc([128, 512], F32) for _ in range(2)]
        for tb in range(4):
            s_ = tb % 2
            t0 = hf * TH + tb * 512
            dma("sp", xb[s_][:], xT[:, :, t0:t0 + 512].rearrange("k p t -> p k t"), "pn_in%d" % s_, w=[("xb", s_)])
            stats_block(xb[s_], ("xb", s_), (sqb[s_], ("sqa", s_)), rstd[s_][:], ("rstd", s_), D)
            for k in range(16):
                eng = "dve"
                P.add(eng, lambda e, k=k, s_=s_, tb=tb: e.scalar_tensor_tensor(
                    out=dst[:, k, tb * 512:(tb + 1) * 512], in0=xb[s_][:, k, :], scalar=gains[:, norm_idx, l, k:k + 1],
                    in1=rstd[s_][:], op0=ALU.mult, op1=ALU.mult), r=[("xb", s_), ("rstd", s_)], w=[(dst_key, k, tb)])
        sb.off = mark
        P.barrier()

    def phase_postnorm(hf, l, norm_idx):
        sb.off = PH0
        xb = [sb.alloc([128, 16, 512], F32) for _ in range(2)]
        mb = [sb.alloc([128, 16, 512], F32) for _ in range(2)]
        sqb = [sb.alloc([128, 16, 512], BF16) for _ in range(2)]
        rstd = [sb.alloc([128, 512], F32) for _ in range(2)]
        for tb in range(4):
            s_ = tb % 2
            t0 = hf * TH + tb * 512
            dma("sp", mb[s_][:], mixT[:, :, t0:t0 + 512].rearrange("k p t -> p k t"), "po_m%d" % s_, w=[("mb", s_)])
            dma("sp", xb[s_][:], xT[:, :, t0:t0 + 512].rearrange("k p t -> p k t"), "po_x%d" % s_, w=[("xb", s_)])
            stats_block(mb[s_], ("mb", s_), (sqb[s_], ("sqa", s_)), rstd[s_][:], ("rstd", s_), D)
            for k in range(16):
                eng = "pool"
                P.add("dve", lambda e, k=k, s_=s_: e.scalar_tensor_tensor(
                    out=mb[s_][:, k, :], in0=mb[s_][:, k, :], scalar=gains[:, norm_idx, l, k:k + 1],
                    in1=rstd[s_][:], op0=ALU.mult, op1=ALU.mult), r=[("mb", s_), ("rstd", s_)], w=[("mb", s_)])
                P.add(eng, lambda e, k=k, s_=s_: e.tensor_tensor(
                    out=xb[s_][:, k, :], in0=xb[s_][:, k, :], in1=mb[s_][:, k, :], op=ALU.add), r=[("mb", s_), ("xb", s_)], w=[("xb", s_)])
            dma("sp", xT[:, :, t0:t0 + 512].rearrange("k p t -> p k t"), xb[s_][:], "po_o%d" % s_, r=[("xb", s_)])
        P.barrier()

    wld = [0]

    def load_w(wbuf, slot, src_ap):
        wld[0] += 1
        return dma("pool", wbuf[slot][:], src_ap.rearrange("(k p) c -> p k c", p=128), "w%d" % slot, w=[("wbuf", slot)])

    def phase_inproj(hf, l):
        sb.off = PH0
        aT = sb.alloc([128, 16, TH], BF16)
        phase_prenorm(hf, l, 0, aT, "aT")
        wbuf = [sb.alloc([128, 16, 512], BF16) for _ in range(2)]
        stf = [sb.alloc([128, TH], F32) for _ in range(2)]
        stb = [sb.alloc([128, TH], BF16) for _ in range(2)]
        aT_keys = [("aT", k, tb) for k in range(16) for tb in range(4)]
        nst = [0]
        h0 = hf * TH
        for cb in range(28):
            slot = cb % 2
            load_w(wbuf, slot, w_in[l, :, cb * 512:(cb + 1) * 512])
            seg = cb // 2 if cb < 16 else 8
            if seg in (3, 6):
                dst = vd if seg == 3 else vr
                c0 = (cb % 2) * 512
                for tg in range(4):
                    si = nst[0] % 2
                    nst[0] += 1
                    for j in range(4):
                        tt = tg * 4 + j
                        b = bank()

                        def mm(e, tt=tt, b=b, slot=slot):
                            ins = None
                            for k in range(16):
                                ins = e.matmul(ps[:, b, :], lhsT=aT[:, k, tt * 128:(tt + 1) * 128], rhs=wbuf[slot][:, k, :], start=(k == 0), stop=(k == 15))
                            return ins
                        P.add("pe", mm, r=aT_keys[tt // 4::4] + [("wbuf", slot)], w=[PS(b)])
                        evac_copy(stb[si][:, j * 512:(j + 1) * 512], ps[:, b, :], r=[PS(b)], w=[("stb", si, j)])
                    r0 = h0 + tg * 512
                    dma("sp", dst[r0:r0 + 512, c0:c0 + 512].rearrange("(j p) c -> p j c", p=128),
                        stb[si][:].rearrange("p (j c) -> p j c", j=4), "ip_sb%d" % si, r=[("stb", si, j) for j in range(4)])
                continue
            for sub in range(4):
                col = cb * 512 + sub * 128
                si = nst[0] % 2
                nst[0] += 1
                for tb in range(4):
                    b = bank()

                    def mm(e, tb=tb, b=b, slot=slot, sub=sub):
                        ins = None
                        for k in range(16):
                            ins = e.matmul(ps[:, b, :], lhsT=wbuf[slot][:, k, sub * 128:(sub + 1) * 128], rhs=aT[:, k, tb * 512:(tb + 1) * 512], start=(k == 0), stop=(k == 15))
                        return ins
                    P.add("pe", mm, r=aT_keys[tb::4] + [("wbuf", slot)], w=[PS(b)])
                    if seg == 0:
                        evac_copy(stf[si][:, tb * 512:(tb + 1) * 512], ps[:, b, :], r=[PS(b)], w=[("stf", si, tb)])
                    elif seg == 7:
                        P.add("act", lambda e, si=si, tb=tb, b=b: e.activation(out=stb[si][:, tb * 512:(tb + 1) * 512], in_=ps[:, b, :], func=AF.Silu), r=[PS(b)], w=[("stb", si, tb)])
                    elif seg == 8:
                        P.add("act", lambda e, si=si, tb=tb, b=b: e.activation(out=stb[si][:, tb * 512:(tb + 1) * 512], in_=ps[:, b, :], func=AF.Sigmoid), r=[PS(b)], w=[("stb", si, tb)])
                    else:
                        evac_copy(stb[si][:, tb * 512:(tb + 1) * 512], ps[:, b, :], r=[PS(b)], w=[("stb", si, tb)])
                if seg == 0:
                    dma("sp", uT[col // 128, :, h0:h0 + TH], stf[si][:], "ip_sf%d" % si, r=[("stf", si, tb) for tb in range(4)])
                else:
                    if seg == 8:
                        dstap = gaT[(col - 8192) // 128, :, h0:h0 + TH]
                    else:
                        dt_ = {1: qdT, 2: kdT, 4: qrT, 5: krT, 7: grT}[seg]
                        dstap = dt_[(col - seg * 1024) // 128, :, h0:h0 + TH]
                    dma("sp", dstap, stb[si][:], "ip_sb%d" % si, r=[("stb", si, tb) for tb in range(4)])
        P.barrier()

    def phase_pool(hf, l):
        sb.off = PH0
        gtl = gtl_all[l % 2]
        pw = sb.alloc([128, 4, 2, 256], BF16)
        corr = sb.alloc([128, 4, 16], F32)
        uas = [sb.alloc([128, 2, 16 + TH], F32) for _ in range(2)]
        ubs = [sb.alloc([128, 2, 16 + TH], F32) for _ in range(2)]
        ucs = [sb.alloc([128, 2, 16 + TH], F32) for _ in range(2)]
        dls = [sb.alloc([128, 2, TH], BF16) for _ in range(2)]
        stb = [sb.alloc([128, TH], BF16) for _ in range(2)]
        h0 = hf * TH
        for g in range(4):
            dma("pool", pw[:, g], pool_w[l, g].rearrange("(k p) c -> p k c", p=128), "pl_w", w=[("pw", g)])
        dma("sp", corr[:].rearrange("p a b -> p (a b)"), c_corr[:, :], "pl_c", w=["corr"])
        nst = 0

        def load_group(g):
            par = g % 2
            ua = uas[par]
            dma("sp", ua[:, :, 0:16], gtl[0:128, g * 32:(g + 1) * 32].rearrange("p (k t) -> p k t", k=2), "pl_u%d" % par, w=[("ua", par)])
            dma("sp", ua[:, :, 16:], uT[2 * g:2 * g + 2, :, 0:TH].rearrange("k p t -> p k t"), "pl_u%d" % par, w=[("ua", par)])
        load_group(0)
        for g in range(4):
            par = g % 2
            ua, ub, uc, dl = uas[par], ubs[par], ucs[par], dls[par]
            uak, dlk = ("ua", par), ("dl", par)
            if g + 1 < 4:
                load_group(g + 1)
            P.add("dve", lambda e, ua=ua: e.tensor_scalar_mul(out=ua[:, :, 0:16], in0=ua[:, :, 0:16], scalar1=hflag[:, 0:1]), r=[uak, "hflag"], w=[uak])
            src, skey = ua, uak
            bufs = [(ub, ("ub", par)), (uc, ("uc", par))]
            step = 1
            for lev in range(g + 1):
                dstb, dkey = bufs[lev % 2]
                P.add("dve", lambda e, src=src, dstb=dstb, step=step: e.tensor_tensor(
                    out=dstb[:, :, 16:], in0=src[:, :, 16:], in1=src[:, :, 16 - step:16 + TH - step], op=ALU.add), r=[skey], w=[dkey])
                if lev < g:
                    P.add("pool", lambda e, src=src, dstb=dstb, step=step: e.tensor_tensor(
                        out=dstb[:, :, step:16], in0=src[:, :, step:16], in1=src[:, :, 0:16 - step], op=ALU.add), r=[skey], w=[dkey])
                src, skey = dstb, dkey
                step *= 2
            wlen = 2 ** (g + 1)
            P.add("dve", lambda e, src=src, g=g: e.tensor_tensor(
                out=src[:, :, 16:32], in0=src[:, :, 16:32], in1=corr[:, g, :].unsqueeze(1).to_broadcast([128, 2, 16]), op=ALU.mult),
                r=[skey, "corr"], w=[skey])
            P.add("dve", lambda e, src=src, wlen=wlen, ua=ua, dl=dl: e.scalar_tensor_tensor(
                out=dl[:], in0=src[:, :, 16:], scalar=1.0 / wlen, in1=ua[:, :, 16:], op0=ALU.mult, op1=ALU.subtract), r=[skey, uak], w=[dlk])
            for dblk in range(2):
                si = nst % 2
                nst += 1
                for tb in range(4):
                    b = bank()

                    def mm(e, g=g, dblk=dblk, tb=tb, b=b, dl=dl):
                        ins = None
                        for c in range(2):
                            ins = e.matmul(ps[:, b, :], lhsT=pw[:, g, c, dblk * 128:(dblk + 1) * 128], rhs=dl[:, c, tb * 512:(tb + 1) * 512], start=(c == 0), stop=(c == 1))
                        return ins
                    P.add("pe", mm, r=[dlk, ("pw", g)], w=[PS(b)])
                    ch = g * 2 + dblk
                    P.add("act", lambda e, si=si, tb=tb, b=b, ch=ch: e.activation(
                        out=stb[si][:, tb * 512:(tb + 1) * 512], in_=ps[:, b, :], func=AF.Copy, scale=pscale[:, l, ch:ch + 1]), r=[PS(b)], w=[("stb", si, tb)])
                dma("sp", yT[g * 2 + dblk, :, h0:h0 + TH], stb[si][:], "pl_o%d" % si, r=[("stb", si, tb) for tb in range(4)])
        P.barrier()

    def phase_diff(hf, l):
        sb.off = PH0
        gk, gv = gk_all[l % 2], gv_all[l % 2]
        wb = sb.alloc([128, 8, 1024], F32)
        wbh = sb.alloc([128, 8, 512], F32)
        nk = 2 * TH
        kT = [sb.alloc([128, nk], BF16) for _ in range(2)]
        vv = [sb.alloc([128, nk // 128, 128], BF16) for _ in range(2)]
        qA = [sb.alloc([128, TH], BF16) for _ in range(2)]
        qB = [sb.alloc([128, TH], BF16) for _ in range(2)]
        NE, LA = 3, 2
        et = [[sb.alloc([128, 512], BF16) for _ in range(NE)] for _ in range(2)]
        tmp = [sb.alloc([128, 512], F32) for _ in range(2)]
        ocp = [[sb.alloc([128, 512], F32) for _ in range(4)] for _ in range(2)]
        sqs = [sb.alloc([128, 512], BF16) for _ in range(2)]
        rss = [sb.alloc([128, 512], F32) for _ in range(2)]
        yst = [sb.alloc([128, 512], BF16) for _ in range(2)]
        h0 = 0
        pending = [None]
        blk_cnt = [0]
        dma("sp", wb[:].rearrange("p a b -> p (a b)"), c_wbias[:, :], "df_wb", w=["wb"])
        dma("sp", wbh[:].rearrange("p a b -> p (a b)"), c_wbiash[:, :], "df_wb", w=["wbh"])
        for s_ in range(2):
            P.add("pool", lambda e, s_=s_: e.memset(qA[s_][64:128, :], 0.0), w=[("qA", s_)])
            P.add("pool", lambda e, s_=s_: e.memset(qB[s_][0:64, :], 0.0), w=[("qB", s_)])
        B_S = [0, 1, 2, 3]
        B_O = [4, 5]
        B_R = [6, 7]
        scnt = 0
        ycnt = 0
        def load_head(h):
            s_ = h % 2
            dma("sp", kT[s_][:, 0:TH], gk[h // 4][0:1024, :].rearrange("(k p x) t -> k p (x t)", k=4, p=128, x=2)[h % 4], "df_k%d" % s_, w=[("kT", s_)])
            dma("sp", kT[s_][:, TH:2 * TH], kdT[h, :, :], "df_k%d" % s_, w=[("kT", s_)])
            for j in range(2):
                dma("sp", vv[s_][:, j * 8:(j + 1) * 8, :], gv[j][0:1024, h * 128:(h + 1) * 128].rearrange("(c p) d -> p c d", p=128), "df_v%d" % s_, w=[("vv", s_)])
            dma("sp", vv[s_][:, 16:32, :], vd[:, h * 128:(h + 1) * 128].rearrange("(c p) d -> p c d", p=128), "df_v%d" % s_, w=[("vv", s_)])
            dma("sp", qA[s_][0:64, :], qdT[h, 0:64, h0:h0 + TH], "df_q%d" % s_, w=[("qA", s_)])
            dma("sp", qB[s_][64:128, :], qdT[h, 64:128, h0:h0 + TH], "df_q%d" % s_, w=[("qB", s_)])
        load_head(0)
        for h in range(8):
            s_ = h % 2
            if h + 1 < 8:
                load_head(h + 1)
            for qb in range(4):
                q0 = TH + qb * 512
                nkc = (q0 + 512) // 128

                def issue_S(kc, qb=qb, q0=q0, h=h, s_=s_):
                    r_ = q0 - kc * 128
                    ei = kc % NE
                    for m in range(2):
                        b = B_S[(kc % 2) * 2 + m]
                        qq = qA if m == 0 else qB
                        P.add("pe", lambda e, b=b, qq=qq, kc=kc: e.matmul(
                            ps[:, b, :], lhsT=kT[s_][:, kc * 128:(kc + 1) * 128], rhs=qq[s_][:, qb * 512:(qb + 1) * 512], start=True, stop=True),
                            r=[("kT", s_), ("qA" if m == 0 else "qB", s_)], w=[PS(b)])
                        ek = ("et", m, ei)
                        hist = kc < 16
                        if r_ >= 256:
                            bt = bfarh if hist else bfar
                            P.add("act", lambda e, b=b, m=m, ei=ei, bt=bt: e.activation(
                                out=et[m][ei][:], in_=ps[:, b, :], func=AF.Exp, scale=0.125, bias=bt[:, h:h + 1]), r=[PS(b)], w=[ek])
                        else:
                            c0 = r_ + 384
                            if hist:
                                assert c0 == 512
                                tab = wbh[:, h, 0:512]
                            else:
                                tab = wb[:, h, c0:c0 + 512]
                            P.add("dve", lambda e, b=b, m=m, tab=tab: e.scalar_tensor_tensor(
                                out=tmp[m][:], in0=ps[:, b, :], scalar=0.125, in1=tab, op0=ALU.mult, op1=ALU.add),
                                r=[PS(b), "wb", "wbh"], w=[("tmp", m)])
                            P.add("act", lambda e, m=m, ei=ei: e.activation(out=et[m][ei][:], in_=tmp[m][:], func=AF.Exp), r=[("tmp", m)], w=[ek])

                def issue_PV(kc, nkc=nkc, h=h, s_=s_):
                    ei = kc % NE
                    for m in range(2):
                        ek = ("et", m, ei)
                        P.add("pe", lambda e, m=m, ei=ei, kc=kc: e.matmul(
                            ps[:, B_O[m], :], lhsT=vv[s_][:, kc, :], rhs=et[m][ei][:], start=(kc == 0), stop=(kc == nkc - 1)),
                            r=[ek, ("vv", s_)], w=[PS(B_O[m])])
                        P.add("pe", lambda e, m=m, ei=ei, kc=kc: e.matmul(
                            ps[:, B_R[m], :], lhsT=onesb[:], rhs=et[m][ei][:], start=(kc == 0), stop=(kc == nkc - 1)),
                            r=[ek], w=[PS(B_R[m])])
                for i in range(nkc + LA):
                    if i < nkc:
                        issue_S(i)
                    if i >= LA:
                        issue_PV(i - LA)
                    if i == 5 and pending[0] is not None:
                        pending[0]()
                        pending[0] = None
                par = blk_cnt[0] % 2
                blk_cnt[0] += 1
                o1c, o2c, r1c, r2c = ocp[par]
                P.add("act", lambda e, o1c=o1c: e.activation(out=o1c[:], in_=ps[:, B_O[0], :], func=AF.Copy), r=[PS(B_O[0])], w=[("oc", par, 0)])
                P.add("dve", lambda e, r1c=r1c: e.tensor_copy(out=r1c[:], in_=ps[:, B_R[0], :]), r=[PS(B_R[0])], w=[("oc", par, 2)])
                P.add("act", lambda e, o2c=o2c: e.activation(out=o2c[:], in_=ps[:, B_O[1], :], func=AF.Copy), r=[PS(B_O[1])], w=[("oc", par, 1)])
                P.add("dve", lambda e, r2c=r2c: e.tensor_copy(out=r2c[:], in_=ps[:, B_R[1], :]), r=[PS(B_R[1])], w=[("oc", par, 3)])

                def part_b(h=h, qb=qb, par=par, o1c=o1c, o2c=o2c, r1c=r1c, r2c=r2c):
                    sq, rs = sqs[par], rss[par]
                    P.add("dve", lambda e: e.reciprocal(out=r1c[:], in_=r1c[:]), r=[("oc", par, 2)], w=[("oc", par, 2)])
                    P.add("dve", lambda e: e.reciprocal(out=r2c[:], in_=r2c[:]), r=[("oc", par, 3)], w=[("oc", par, 3)])
                    P.add("dve", lambda e: e.tensor_tensor(out=o1c[:], in0=o1c[:], in1=r1c[:], op=ALU.mult), r=[("oc", par, 0), ("oc", par, 2)], w=[("oc", par, 0)])
                    P.add("pool", lambda e: e.tensor_tensor(out=o2c[:], in0=o2c[:], in1=r2c[:], op=ALU.mult), r=[("oc", par, 1), ("oc", par, 3)], w=[("oc", par, 1)])
                    P.add("dve", lambda e: e.scalar_tensor_tensor(out=o1c[:], in0=o2c[:], scalar=neglam[:, l:l + 1], in1=o1c[:], op0=ALU.mult, op1=ALU.add),
                          r=[("oc", par, 0), ("oc", par, 1), "neglam"], w=[("oc", par, 0)])
                    P.add("act", lambda e: e.activation(out=sq[:], in_=o1c[:], func=AF.Square), r=[("oc", par, 0)], w=[("sq", par)])
                    b = B_S[0]
                    P.add("pe", lambda e: e.matmul(ps[:, b, :], lhsT=onesb[:], rhs=sq[:], start=True, stop=True), r=[("sq", par)], w=[PS(b)])
                    P.add("act", lambda e: e.activation(out=rs[:], in_=ps[:, b, :], func=AF.Sqrt, scale=1.0 / 128, bias=epsc[:]), r=[PS(b)], w=[("rs", par)])
                    P.add("dve", lambda e: e.reciprocal(out=rs[:], in_=rs[:]), r=[("rs", par)], w=[("rs", par)])
                    P.add("dve", lambda e: e.tensor_scalar(out=rs[:], in0=rs[:], scalar1=hgain[:, l:l + 1], scalar2=1.0 - lambda_init(l), op0=ALU.mult, op1=ALU.mult),
                          r=[("rs", par), "hgain"], w=[("rs", par)])
                    P.add("pool", lambda e: e.tensor_tensor(out=yst[par][:], in0=o1c[:], in1=rs[:], op=ALU.mult), r=[("oc", par, 0), ("rs", par)], w=[("yst", par)])
                    dma("sp", yT[8 + h, :, qb * 512:(qb + 1) * 512], yst[par][:], "df_o%d" % par, r=[("yst", par)])
                pending[0] = part_b
        if pending[0] is not None:
            pending[0]()
            pending[0] = None
        P.barrier()

    def phase_ret(hf, l, state_only=False):
        sb.off = PH0
        gst = gst_all[l % 2]
        cosb = sb.alloc([128, TH], F32)
        sinb = sb.alloc([128, TH], F32)
        dec = sb.alloc([128, 8, 2, 128], F32)
        mask = sb.alloc([128, 512], F32)
        raw = [[sb.alloc([128, TH], BF16) for _ in range(2)] for _ in range(2)]
        vt = [sb.alloc([128, 16, 128], BF16) for _ in range(2)]
        gt = [sb.alloc([128, TH], BF16) for _ in range(2)]
        rot = [sb.alloc([128, TH], BF16) for _ in range(2)]
        ktok = sb.alloc([128, 16, 128], BF16)
        stbf = sb.alloc([128, 16, 128], BF16)
        st32s = [sb.alloc([128, 128], F32) for _ in range(2)]
        PA = sb.alloc([128, 16, 128], F32)
        PB = sb.alloc([128, 16, 128], F32)
        Ug = sb.alloc([128, 16, 128], F32)
        t1 = [sb.alloc([128, 512], F32) for _ in range(2)]
        t2 = [sb.alloc([128, 512], F32) for _ in range(2)]
        mt = [sb.alloc([128, 512], BF16) for _ in range(2)]
        sqs = [sb.alloc([128, 512], BF16) for _ in range(2)]
        rss = [sb.alloc([128, 512], F32) for _ in range(2)]
        o32s = [sb.alloc([128, 512], F32) for _ in range(2)]
        yst = [sb.alloc([128, 512], BF16) for _ in range(2)]
        h0 = hf * TH
        dma("sp", cosb[:], c_cos[:, h0:h0 + TH], "rt_c", w=["cos"])
        dma("sp", sinb[:], c_sin[:, h0:h0 + TH], "rt_c", w=["sin"])
        dma("sp", dec[:].rearrange("p a b c -> p (a b c)"), c_dec[:, :], "rt_c", w=["dec"])
        dma("sp", mask[:], c_mask[:, :], "rt_c", w=["mask"])
        gam = [1.0 - 2.0 ** (-5.0 - h) for h in range(8)]
        tcnt = 0
        ycnt = 0
        def load_head(h):
            s_ = h % 2
            if not state_only:
                dma("sp", raw[s_][0][:], qrT[h, :, h0:h0 + TH], "rt_q%d" % s_, w=[("raw", s_, 0)])
                dma("sp", gt[s_][:], grT[h, :, h0:h0 + TH], "rt_g%d" % s_, w=[("gt", s_)])
                dma("sp", st32s[s_][:], gst[h * 128:(h + 1) * 128, :], "rt_s%d" % s_, w=[("st32", s_)])
            dma("sp", raw[s_][1][:], krT[h, :, h0:h0 + TH], "rt_k%d" % s_, w=[("raw", s_, 1)])
            dma("sp", vt[s_][:], vr[h0:h0 + TH, h * 128:(h + 1) * 128].rearrange("(c p) d -> p c d", p=128), "rt_v%d" % s_, w=[("vt", s_)])
        load_head(0)
        for h in range(8):
            s_ = h % 2
            st32 = st32s[s_]
            if h + 1 < 8:
                load_head(h + 1)
            if not state_only:
                P.add("dve", lambda e, st32=st32: e.tensor_scalar_mul(out=st32[:], in0=st32[:], scalar1=hflag[:, 0:1]), r=[("st32", s_), "hflag"], w=[("st32", s_)])
            for t in ((1,) if state_only else (0, 1)):
                for tb in range(4):
                    b = bank()
                    ti = tcnt % 2
                    tcnt += 1
                    sl = slice(tb * 512, (tb + 1) * 512)
                    P.add("pe", lambda e, b=b, s_=s_, t=t, sl=sl: e.matmul(ps[:, b, :], lhsT=swapb[:], rhs=raw[s_][t][:, sl], start=True, stop=True),
                          r=[("raw", s_, t)], w=[PS(b)])
                    P.add("pool", lambda e, ti=ti, s_=s_, t=t, sl=sl: e.tensor_tensor(out=t1[ti][:], in0=raw[s_][t][:, sl], in1=cosb[:, sl], op=ALU.mult),
                          r=[("raw", s_, t), "cos"], w=[("t1", ti)])
                    P.add("dve", lambda e, ti=ti, b=b, sl=sl: e.tensor_tensor(out=t2[ti][:], in0=ps[:, b, :], in1=sinb[:, sl], op=ALU.mult),
                          r=[PS(b), "sin"], w=[("t2", ti)])
                    P.add("pool", lambda e, ti=ti: e.tensor_tensor(out=t1[ti][:], in0=t1[ti][:], in1=t2[ti][:], op=ALU.add),
                          r=[("t1", ti), ("t2", ti)], w=[("t1", ti)])
                    P.add("dve", lambda e, ti=ti, t=t, sl=sl, h=h: e.tensor_tensor(
                        out=rot[t][:, sl].rearrange("p (a b) -> p a b", a=4), in0=t1[ti][:].rearrange("p (a b) -> p a b", a=4),
                        in1=dec[:, h, t, :].unsqueeze(1).to_broadcast([128, 4, 128]), op=ALU.mult),
                        r=[("t1", ti), "dec"], w=[("rot", t, tb)])
            rotk = [("rot", 1, tb) for tb in range(4)]
            rotq = [("rot", 0, tb) for tb in range(4)]
            for cg in range(4):
                b = bank()

                def tr(e, b=b, cg=cg):
                    ins = None
                    pb = ps[:, b, :].bitcast(BF16)
                    for j in range(4):
                        c = cg * 4 + j
                        ins = e.transpose(out=pb[:, j * 128:(j + 1) * 128], in_=rot[1][:, c * 128:(c + 1) * 128], identity=identb[:])
                    return ins
                P.add("pe", tr, r=[("rot", 1, cg)], w=[PS(b)])
                P.add("act", lambda e, b=b, cg=cg: e.activation(out=ktok[:, cg * 4:(cg + 1) * 4, :].rearrange("p a b -> p (a b)"),
                                                                in_=ps[:, b, :].bitcast(BF16)[:, 0:512], func=AF.Copy), r=[PS(b)], w=[("ktok", cg)])
            kb = []
            for cg in range(4):
                b = bank()
                kb.append(b)

                def mmk(e, b=b, cg=cg, s_=s_):
                    ins = None
                    for j in range(4):
                        c = cg * 4 + j
                        ins = e.matmul(ps[:, b, j * 128:(j + 1) * 128], lhsT=ktok[:, c, :], rhs=vt[s_][:, c, :], start=True, stop=True)
                    return ins
                P.add("pe", mmk, r=[("ktok", cg), ("vt", s_)], w=[PS(b)])
            g128 = gam[h] ** 128
            for cg in range(4):
                evac_copy(PA[:, cg * 4:(cg + 1) * 4, :].rearrange("p a b -> p (a b)"), ps[:, kb[cg], :], r=[PS(kb[cg])], w=["PA"])
            src, skey, dstb, dkey = PA, "PA", PB, "PB"
            for sh in (1, 2, 4, 8):
                gs = g128 ** sh
                P.add("dve", lambda e, src=src, dstb=dstb, sh=sh, gs=gs: e.scalar_tensor_tensor(
                    out=dstb[:, sh:16, :], in0=src[:, 0:16 - sh, :], scalar=gs, in1=src[:, sh:16, :], op0=ALU.mult, op1=ALU.add), r=[skey], w=[dkey])
                P.add("pool", lambda e, src=src, dstb=dstb, sh=sh: e.tensor_copy(out=dstb[:, 0:sh, :], in_=src[:, 0:sh, :]), r=[skey], w=[dkey])
                src, skey, dstb, dkey = dstb, dkey, src, skey
            if state_only:
                P.add("dve", lambda e, src=src, st32=st32, g128=g128: e.tensor_scalar_mul(out=st32[:], in0=src[:, 15, :], scalar1=g128), r=[skey], w=[("st32", s_)])
                dma("sp", rst[h], st32[:], "rt_so", r=[("st32", s_)])
                continue
            for c in range(1, 16):
                P.add("dve", lambda e, c=c, st32=st32, g128=g128: e.tensor_scalar_mul(out=Ug[:, c, :], in0=st32[:], scalar1=g128 ** c), r=[("st32", s_)], w=[("Ug", c)])
            P.add("act", lambda e, st32=st32: e.activation(out=stbf[:, 0, :], in_=st32[:], func=AF.Copy), r=[("st32", s_)], w=["stbf0"])
            P.add("dve", lambda e, src=src, g128=g128: e.scalar_tensor_tensor(
                out=stbf[:, 1:16, :], in0=src[:, 0:15, :], scalar=g128, in1=Ug[:, 1:16, :], op0=ALU.mult, op1=ALU.add),
                r=[skey] + [("Ug", c) for c in range(1, 16)], w=["stbf"])
            for cg in range(4):
                b = bank()
                mi = cg % 2

                def mms(e, b=b, cg=cg):
                    ins = None
                    for j in range(4):
                        c = cg * 4 + j
                        ins = e.matmul(ps[:, b, j * 128:(j + 1) * 128], lhsT=rot[1][:, c * 128:(c + 1) * 128], rhs=rot[0][:, c * 128:(c + 1) * 128], start=True, stop=True)
                    return ins
                P.add("pe", mms, r=[("rot", 1, cg), ("rot", 0, cg)], w=[PS(b)])
                P.add("dve", lambda e, b=b, mi=mi: e.tensor_tensor(out=mt[mi][:], in0=ps[:, b, :], in1=mask[:], op=ALU.mult), r=[PS(b), "mask"], w=[("mt", mi)])
                bo = bank()

                def mmo(e, bo=bo, cg=cg, mi=mi, s_=s_):
                    ins = None
                    for j in range(4):
                        c = cg * 4 + j
                        e.matmul(ps[:, bo, j * 128:(j + 1) * 128], lhsT=vt[s_][:, c, :], rhs=mt[mi][:, j * 128:(j + 1) * 128], start=True, stop=False)
                        ins = e.matmul(ps[:, bo, j * 128:(j + 1) * 128], lhsT=stbf[:, c, :], rhs=rot[0][:, c * 128:(c + 1) * 128], start=False, stop=True)
                    return ins
                P.add("pe", mmo, r=[("mt", mi), ("vt", s_), "stbf", "stbf0", ("rot", 0, cg)], w=[PS(bo)])
                sq, rs, o32 = sqs[mi], rss[mi], o32s[mi]
                P.add("act", lambda e, bo=bo, sq=sq: e.activation(out=sq[:], in_=ps[:, bo, :], func=AF.Square), r=[PS(bo)], w=[("sq", mi)])
                bn = bank()
                P.add("pe", lambda e, bn=bn, sq=sq: e.matmul(ps[:, bn, :], lhsT=onesb[:], rhs=sq[:], start=True, stop=True), r=[("sq", mi)], w=[PS(bn)])
                P.add("act", lambda e, bn=bn, rs=rs: e.activation(out=rs[:], in_=ps[:, bn, :], func=AF.Sqrt, scale=1.0 / 128, bias=epsc[:]), r=[PS(bn)], w=[("rs", mi)])
                P.add("dve", lambda e, rs=rs: e.reciprocal(out=rs[:], in_=rs[:]), r=[("rs", mi)], w=[("rs", mi)])
                P.add("dve", lambda e, bo=bo, rs=rs, o32=o32: e.tensor_tensor(out=o32[:], in0=ps[:, bo, :], in1=rs[:], op=ALU.mult), r=[PS(bo), ("rs", mi)], w=[("o32", mi)])
                yi = ycnt % 2
                ycnt += 1
                P.add("pool", lambda e, yi=yi, s_=s_, cg=cg, o32=o32: e.tensor_tensor(out=yst[yi][:], in0=o32[:], in1=gt[s_][:, cg * 512:(cg + 1) * 512], op=ALU.mult),
                      r=[("o32", mi), ("gt", s_)], w=[("yst", yi)])
                t0 = h0 + cg * 512
                dma("sp", yT[16 + h, :, t0:t0 + 512], yst[yi][:], "rt_o%d" % yi, r=[("yst", yi)])
        P.barrier()

    def phase_merge(hf, l):
        sb.off = PH0
        yr = sb.alloc([128, 24, TH], BF16)
        wbuf = [sb.alloc([128, 24, 256], BF16) for _ in range(2)]
        gb = [sb.alloc([128, 3, 512], BF16) for _ in range(2)]
        m0 = [sb.alloc([128, 512], F32) for _ in range(2)]
        m1 = [sb.alloc([128, 512], F32) for _ in range(2)]
        m2 = [sb.alloc([128, 512], F32) for _ in range(2)]
        stb = [sb.alloc([128, TH], BF16) for _ in range(2)]
        h0 = hf * TH
        for n in range(3):
            dma("sp", yr[:, n * 8:(n + 1) * 8, :], yT[n * 8:(n + 1) * 8, :, h0:h0 + TH].rearrange("k p t -> p k t"), "mg_y%d" % n, w=[("yr", n)])
        gcnt = 0
        def ldw(wbk):
            slot = wbk % 2
            for n in range(3):
                dma("pool", wbuf[slot][:, n * 8:(n + 1) * 8, :], w_branch[l, n, :, wbk * 256:(wbk + 1) * 256].rearrange("(k p) c -> p k c", p=128),
                    "w%d" % slot, w=[("wbuf", slot, n)])
        ldw(0)
        for wbk in range(8):
            slot = wbk % 2
            if wbk + 1 < 8:
                ldw(wbk + 1)
            for sub in range(2):
                dblk = wbk * 2 + sub
                si = dblk % 2
                for tb in range(4):
                    gi = gcnt % 2
                    gcnt += 1
                    t0 = h0 + tb * 512
                    for n in range(3):
                        dma("sp", gb[gi][:, n, :], gaT[n * 16 + dblk, :, t0:t0 + 512], "mg_g%d" % gi, w=[("gb", gi, n)])
                    bs = []
                    for n in range(3):
                        b = bank()
                        bs.append(b)

                        def mm(e, b=b, n=n, slot=slot, sub=sub, tb=tb):
                            ins = None
                            for c in range(8):
                                ins = e.matmul(ps[:, b, :], lhsT=wbuf[slot][:, n * 8 + c, sub * 128:(sub + 1) * 128], rhs=yr[:, n * 8 + c, tb * 512:(tb + 1) * 512], start=(c == 0), stop=(c == 7))
                            return ins
                        P.add("pe", mm, r=[("wbuf", slot, n), ("yr", n)], w=[PS(b)])
                    mm_ = (m0[gi], m1[gi], m2[gi])
                    for n in range(3):
                        P.add("dve", lambda e, n=n, gi=gi, b=bs[n], mm_=mm_: e.tensor_tensor(out=mm_[n][:], in0=ps[:, b, :], in1=gb[gi][:, n, :], op=ALU.mult),
                              r=[PS(bs[n]), ("gb", gi, n)], w=[("m", gi, n)])
                    P.add("pool", lambda e, mm_=mm_: e.tensor_tensor(out=mm_[0][:], in0=mm_[0][:], in1=mm_[1][:], op=ALU.add),
                          r=[("m", gi, 0), ("m", gi, 1)], w=[("m", gi, 0)])
                    P.add("pool", lambda e, mm_=mm_, si=si, tb=tb: e.tensor_tensor(out=stb[si][:, tb * 512:(tb + 1) * 512], in0=mm_[0][:], in1=mm_[2][:], op=ALU.add),
                          r=[("m", gi, 0), ("m", gi, 2)], w=[("stb", si, tb)])
                dma("sp", mgT[dblk, :, h0:h0 + TH], stb[si][:], "mg_o%d" % si, r=[("stb", si, tb) for tb in range(4)])
        P.barrier()

    def proj_fm(hf, l, src_res, src_keys, nk, wsrc, ncol, dst, evac, dst_t0, ntb, tb_off=0, dt_=F32, wnk=None):
        wbuf = [sb.alloc([128, nk, 512], BF16) for _ in range(2)]
        st = [sb.alloc([128, ntb * 512], dt_) for _ in range(2)]
        nst = 0
        for cb in range(ncol // 512):
            slot = cb % 2
            load_w(wbuf, slot, wsrc[:, cb * 512:(cb + 1) * 512])
            for sub in range(4):
                si = nst % 2
                nst += 1
                for tb in range(ntb):
                    b = bank()

                    def mm(e, b=b, slot=slot, sub=sub, tb=tb):
                        ins = None
                        for k in range(nk):
                            ins = e.matmul(ps[:, b, :], lhsT=wbuf[slot][:, k, sub * 128:(sub + 1) * 128], rhs=src_res[:, k, tb * 512:(tb + 1) * 512], start=(k == 0), stop=(k == nk - 1))
                        return ins
                    P.add("pe", mm, r=[("wbuf", slot)] + src_keys(tb), w=[PS(b)])
                    evac(st[si][:, tb * 512:(tb + 1) * 512], b, ("pst", si, tb))
                dma("sp", dst[cb * 4 + sub, :, dst_t0:dst_t0 + ntb * 512], st[si][:], "pj_o%d" % si, r=[("pst", si, tb) for tb in range(ntb)])

    def phase_outproj(hf, l):
        sb.off = PH0
        mr = sb.alloc([128, 16, TH], BF16)
        h0 = hf * TH
        dma("sp", mr[:], mgT[:, :, h0:h0 + TH].rearrange("k p t -> p k t"), "op_m", w=["mr"])

        def evac(o, b, key):
            evac_copy(o, ps[:, b, :], r=[PS(b)], w=[key])
        proj_fm(hf, l, mr, lambda tb: ["mr"], 16, w_out[l], D, mixT, evac, h0, 4)
        P.barrier()

    def phase_mlp(hf, l):
        sb.off = PH0
        hT = sb.alloc([128, 16, TH], BF16)
        phase_prenorm(hf, l, 2, hT, "hT")
        h0 = hf * TH
        mark = sb.off
        sqt = [sb.alloc([128, 512], F32) for _ in range(2)]
        qn = [0]

        def evac(o, b, key):
            qi = qn[0] % 2
            qn[0] += 1
            P.add("act", lambda e, b=b, qi=qi: e.activation(out=sqt[qi][:], in_=ps[:, b, :], func=AF.Square), r=[PS(b)], w=[("sqt", qi)])
            P.add("dve", lambda e, b=b, qi=qi: e.scalar_tensor_tensor(out=o, in0=ps[:, b, :], scalar=0.0, in1=sqt[qi][:], op0=ALU.is_gt, op1=ALU.mult),
                  r=[PS(b), ("sqt", qi)], w=[key])
        proj_fm(hf, l, hT, lambda tb: [("hT", k, tb) for k in range(16)], 16, w_up[l], DFF, ffT, evac, 0, 4, dt_=BF16)
        P.barrier()
        sb.off = PH0
        fr = sb.alloc([128, 64, 1024], BF16)
        wbuf = [sb.alloc([128, 16, 512], BF16) for _ in range(2)]
        st = [sb.alloc([128, 1024], F32) for _ in range(2)]
        wn = 0
        nst = 0
        for tb2 in range(2):
            for kq in range(4):
                dma("sp", fr[:, kq * 16:(kq + 1) * 16, :], ffT[kq * 16:(kq + 1) * 16, :, tb2 * 1024:(tb2 + 1) * 1024].rearrange("k p t -> p k t"),
                    "dn_f%d" % kq, w=[("fr", kq)])
            for eb in range(4):
                for kq in range(4):
                    slot = wn % 2
                    wn += 1
                    load_w(wbuf, slot, w_down[l, kq * 2048:(kq + 1) * 2048, eb * 512:(eb + 1) * 512])
                    for sub in range(4):
                        for t in range(2):
                            b = sub * 2 + t

                            def mm(e, b=b, slot=slot, sub=sub, t=t, kq=kq):
                                ins = None
                                for k in range(16):
                                    ins = e.matmul(ps[:, b, :], lhsT=wbuf[slot][:, k, sub * 128:(sub + 1) * 128], rhs=fr[:, kq * 16 + k, t * 512:(t + 1) * 512],
                                                   start=(kq == 0 and k == 0), stop=(kq == 3 and k == 15))
                                return ins
                            P.add("pe", mm, r=[("wbuf", slot), ("fr", kq)], w=[PS(b)])
                for sub in range(4):
                    si = nst % 2
                    nst += 1
                    for t in range(2):
                        b = sub * 2 + t
                        evac_copy(st[si][:, t * 512:(t + 1) * 512], ps[:, b, :], r=[PS(b)], w=[("dst", si, t)])
                    t0 = h0 + tb2 * 1024
                    dma("sp", mixT[eb * 4 + sub, :, t0:t0 + 1024], st[si][:], "dn_o%d" % si, r=[("dst", si, 0), ("dst", si, 1)])
        P.barrier()

    PAIRS = [[0, 1], [2, 3], [4, 5], [6, 7]]

    def phase_exchange(l, part):
        gk, gv, gst, gtl = gk_all[l % 2], gv_all[l % 2], gst_all[l % 2], gtl_all[l % 2]

        def cc(i, src, dst):
            P.add("pool", lambda e: e.collective_compute("AllGather", ALU.bypass, replica_groups=PAIRS, ins=[src], outs=[dst]),
                  chan="cc%d" % i, inc=1)
        if part == 0:
            dma("sp", utl.rearrange("p (k t) -> p k t", k=8), uT[:, :, TH - 16:TH].rearrange("k p t -> p k t"), "ex_t")
            P.barrier()
            for j in range(2):
                cc(j, kdT[4 * j:4 * j + 4].rearrange("k p (x t) -> (k p x) t", x=2), gk[j][:, :])
                cc(2 + j, vd[j * 1024:(j + 1) * 1024, :], gv[j][:, :])
            cc(5, utl[:, :], gtl[:, :])
        else:
            cc(4, rst.rearrange("h p e -> (h p) e"), gst[:, :])
            P.barrier()

    phase_transpose_in()
    for l in range(n_layers):
        phase_inproj(0, l)
        phase_exchange(l, 0)
        phase_ret(0, l, state_only=True)
        phase_exchange(l, 1)
        phase_pool(0, l)
        phase_diff(0, l)
        phase_ret(0, l)
        phase_merge(0, l)
        phase_outproj(0, l)
        phase_postnorm(0, l, 1)
        phase_mlp(0, l)
        phase_postnorm(0, l, 3)
    phase_transpose_out()
    P.add("sp", None, r=["OUT"])

    P.lower()
    chans = sorted(P.chan_cnt.keys())
    csem = {c: stack.enter_context(nc.semaphore("sc_" + c)) for c in chans}
    block = stack.enter_context(nc.Block())
    P.emit(nc, block, esem, csem)
    stack.close()
    return nc


def t5_bucket(n):
    n = np.maximum(n, 0)
    exact = 16
    nf = np.maximum(n, 1).astype(np.float32)
    large = exact + (np.log(nf / np.float32(exact)) / np.float32(math.log(128 / exact)) * np.float32(32 - exact)).astype(np.int32)
    large = np.minimum(large, 31)
    return np.where(n < exact, n, large)


def make_consts(inputs, hf):
    f32 = np.float32
    c = {}
    g = np.stack([inputs["norm_pre_mix"], inputs["norm_post_mix"], inputs["norm_pre_mlp"], inputs["norm_post_mlp"]], 0)
    c["c_gains"] = np.ascontiguousarray(g.reshape(4, DEPTH, 16, 128).transpose(3, 0, 1, 2).reshape(128, -1)).astype(f32)
    c["c_pscale"] = np.ascontiguousarray(inputs["pool_scale"].reshape(DEPTH, 8, 128).transpose(2, 0, 1).reshape(128, -1)).astype(f32)
    c["c_hgain"] = np.ascontiguousarray(inputs["diff_head_norm"].T).astype(f32)
    c["c_lam"] = np.ascontiguousarray(np.broadcast_to(inputs["diff_lambda"].reshape(1, -1), (128, DEPTH * 256))).astype(f32)
    rb = inputs["rel_bias"].astype(f32)
    c["c_bfar"] = np.ascontiguousarray(np.broadcast_to(rb[31:32, :], (128, 8))).astype(f32)
    p = np.arange(128)[:, None]
    cc = np.arange(1024)[None, :]
    dist = cc - 384 - p
    bk = t5_bucket(dist)
    wb = rb[bk, :]
    wb = np.where((dist >= 0)[:, :, None], wb, f32(-1e30))
    wbt = np.ascontiguousarray(wb.transpose(0, 2, 1)).astype(f32)
    c["c_wbias"] = wbt.reshape(128, -1)
    if hf == 1:
        c["c_wbiash"] = np.ascontiguousarray(wbt[:, :, 512:1024]).reshape(128, -1)
        c["c_bfarh"] = c["c_bfar"].copy()
    else:
        c["c_wbiash"] = np.full((128, 8 * 512), -1e30, f32)
        c["c_bfarh"] = np.full((128, 8), -1e30, f32)
    c["c_hflag"] = np.full((128, 1), float(hf), f32)
    half = 64
    inv_freq = (f32(1.0) / (f32(10000.0) ** np.linspace(0.0, 1.0, half, dtype=f32))).astype(f32)
    ang = (np.arange(S, dtype=f32)[:, None] * inv_freq[None, :]).astype(f32)
    cos = np.cos(ang).astype(f32).T
    sin = np.sin(ang).astype(f32).T
    c["c_cos"] = np.ascontiguousarray(np.concatenate([cos, cos], 0)[:, hf * TH:(hf + 1) * TH])
    c["c_sin"] = np.ascontiguousarray(np.concatenate([-sin, sin], 0)[:, hf * TH:(hf + 1) * TH])
    hh = np.arange(8, dtype=np.float64)
    lg = np.log(1.0 - 2.0 ** (-5.0 - hh))
    i = np.arange(128, dtype=np.float64)
    qd = np.exp((i[None, :] + 1) * lg[:, None])
    kd = np.exp(-(i[None, :] + 1) * lg[:, None]) * (128 ** -0.5)
    dec = np.stack([qd, kd], 1)
    c["c_dec"] = np.ascontiguousarray(np.broadcast_to(dec.reshape(1, -1), (128, 8 * 2 * 128))).astype(f32)
    corr = np.zeros((4, 16), np.float64)
    for gi in range(4):
        w = 2 ** (gi + 1)
        for t in range(16):
            corr[gi, t] = (w / min(t + 1, w)) if hf == 0 else 1.0
    c["c_corr"] = np.ascontiguousarray(np.broadcast_to(corr.reshape(1, -1), (128, 64))).astype(f32)
    j = np.arange(128)[:, None]
    ii = np.arange(512)[None, :] % 128
    c["c_mask"] = (ii >= j).astype(f32)
    c["c_ident"] = np.eye(128, dtype=f32)
    sw = np.zeros((128, 128), f32)
    for m in range(128):
        sw[(m + 64) % 128, m] = 1.0
    c["c_swap"] = sw
    return c


_NC_CACHE = {}


def kernel(**inputs):
    inputs = {k: np.asarray(v) for k, v in inputs.items()}
    if "nc" not in _NC_CACHE:
        _NC_CACHE["nc"] = build_program()
    nc = _NC_CACHE["nc"]
    consts = [make_consts(inputs, 0), make_consts(inputs, 1)]
    shared = {k: np.ascontiguousarray(inputs[k], dtype=np.float32) for k in ("w_in", "pool_w", "w_branch", "w_out", "w_up", "w_down")}
    in_maps = []
    for core in range(8):
        b, hf = core // 2, core % 2
        m = dict(shared)
        m.update(consts[hf])
        m["x"] = np.ascontiguousarray(inputs["x"][b, hf * TH:(hf + 1) * TH], dtype=np.float32)
        in_maps.append(m)
    res = run_bass_kernel_spmd(nc, in_maps, core_ids=list(range(8)))
    full = np.empty((4, S, D), np.float32)
    for core in range(8):
        b, hf = core // 2, core % 2
        full[b, hf * TH:(hf + 1) * TH] = np.asarray(res.results[core]["out"], dtype=np.float32)
    return full
```
